# Optimizing a Trainium2 kernel written in Bass

```python
import jax, jax.numpy as jnp
from jax import lax
import numpy as np

D_MODEL = 1024
BATCH = 4
SEQ = 4096
DEPTH = 1

LRU_WIDTH = D_MODEL // 2
LRU_BLOCKS = 8
LRU_BLOCK_DIM = LRU_WIDTH // LRU_BLOCKS
CONV_WIDTH = 4
RG_C = 8.0
LRU_MIN_RAD = 0.9
LRU_MAX_RAD = 0.999
RET_HEADS = 8
RET_HEAD_DIM = 64
RET_WIDTH = RET_HEADS * RET_HEAD_DIM
CHUNK = 128
ROPE_BASE = 10000.0
D_FF = 4 * D_MODEL
EPS = 1e-6
IN_SPLITS = (LRU_WIDTH, LRU_WIDTH, RET_WIDTH, RET_WIDTH, RET_WIDTH, RET_WIDTH, D_MODEL, D_MODEL)
D_IN = sum(IN_SPLITS)

kernel_name = "hybrid_rglru_retention_gated_block"


def rmsnorm(x, g):
    xf = x.astype(jnp.float32)
    y = xf * lax.rsqrt(jnp.mean(xf * xf, axis=-1, keepdims=True) + EPS)
    return (y * g.astype(jnp.float32)).astype(x.dtype)


def causal_depthwise_conv(x, w, b):
    c = x.shape[-1]
    y = lax.conv_general_dilated(x, w[:, None, :], window_strides=(1,),
                                 padding=[(CONV_WIDTH - 1, 0)],
                                 dimension_numbers=("NWC", "WIO", "NWC"),
                                 feature_group_count=c)
    return y + b


def rg_lru(x, w_r, b_r, w_i, b_i, lam):
    bsz, s, w = x.shape
    xb = x.reshape(bsz, s, LRU_BLOCKS, LRU_BLOCK_DIM)
    r = jax.nn.sigmoid(jnp.einsum("bshd,hde->bshe", xb, w_r) + b_r).reshape(bsz, s, w)
    i = jax.nn.sigmoid(jnp.einsum("bshd,hde->bshe", xb, w_i) + b_i).reshape(bsz, s, w)
    log_a = -RG_C * r.astype(jnp.float32) * jax.nn.softplus(-lam.astype(jnp.float32))
    a = jnp.exp(log_a)
    mult = jnp.sqrt(-jnp.expm1(2.0 * log_a))
    u = mult * (i * x).astype(jnp.float32)

    def combine(c1, c2):
        a1, b1 = c1
        a2, b2 = c2
        return a1 * a2, a2 * b1 + b2

    _, h = lax.associative_scan(combine, (a, u), axis=1)
    return h.astype(x.dtype)


def rotary(x, pos):
    half = x.shape[-1] // 2
    inv = ROPE_BASE ** (-jnp.arange(half, dtype=jnp.float32) / half)
    ang = pos[:, None] * inv[None, :]
    cos = jnp.cos(ang)[None, :, None, :]
    sin = jnp.sin(ang)[None, :, None, :]
    xf = x.astype(jnp.float32)
    x1, x2 = xf[..., :half], xf[..., half:]
    return jnp.concatenate([x1 * cos - x2 * sin, x1 * sin + x2 * cos], axis=-1).astype(x.dtype)


def chunkwise_retention(q, k, v):
    bsz, s, nh, dh = q.shape
    n = s // CHUNK
    log_g = jnp.log1p(-jnp.exp2(-5.0 - jnp.arange(nh, dtype=jnp.float32)))
    pos = jnp.arange(CHUNK, dtype=jnp.float32)
    rel = pos[:, None] - pos[None, :]
    intra_decay = jnp.where(rel[None] >= 0,
                            jnp.exp(log_g[:, None, None] * jnp.maximum(rel, 0.0)[None]), 0.0)
    q_decay = jnp.exp(log_g[:, None] * (pos[None, :] + 1.0))
    k_decay = jnp.exp(log_g[:, None] * (CHUNK - 1.0 - pos[None, :]))
    chunk_decay = jnp.exp(log_g * CHUNK)
    qc = q.astype(jnp.float32).reshape(bsz, n, CHUNK, nh, dh)
    kc = k.astype(jnp.float32).reshape(bsz, n, CHUNK, nh, dh)
    vc = v.astype(jnp.float32).reshape(bsz, n, CHUNK, nh, dh)
    scores = jnp.einsum("bnchd,bnmhd->bnhcm", qc, kc) * intra_decay[None, None]
    intra = jnp.einsum("bnhcm,bnmhe->bnche", scores, vc)
    kv = jnp.einsum("bnchd,hc,bnche->bnhde", kc, k_decay, vc)

    def step(state, kv_i):
        return chunk_decay[None, :, None, None] * state + kv_i, state

    _, states = lax.scan(step, jnp.zeros((bsz, nh, dh, dh), jnp.float32), jnp.moveaxis(kv, 1, 0))
    states = jnp.moveaxis(states, 0, 1)
    inter = jnp.einsum("bnchd,hc,bnhde->bnche", qc, q_decay, states)
    return (intra + inter).reshape(bsz, s, nh, dh)


def setup_inputs(seed: int = 0) -> dict:
    key = jax.random.key(seed)
    ks = jax.random.split(key, 20)
    L = DEPTH

    def nrm(k, shape, fan_in):
        return jax.random.normal(k, shape, jnp.float32) * fan_in ** -0.5

    u = jax.random.uniform(ks[9], (L, LRU_WIDTH), jnp.float32)
    rad2 = u * (LRU_MAX_RAD ** 2 - LRU_MIN_RAD ** 2) + LRU_MIN_RAD ** 2
    a_real = 0.5 * jnp.log(rad2)
    lru_lambda = -jnp.log(jnp.expm1(-a_real))
    return {
        "x": jax.random.normal(ks[0], (BATCH, SEQ, D_MODEL), jnp.float32),
        "norm1_g": 1.0 + 0.05 * jax.random.normal(ks[1], (L, D_MODEL), jnp.float32),
        "w_in": nrm(ks[2], (L, D_MODEL, D_IN), D_MODEL),
        "conv_w": nrm(ks[3], (L, CONV_WIDTH, LRU_WIDTH), CONV_WIDTH),
        "conv_b": 0.02 * jax.random.normal(ks[4], (L, LRU_WIDTH), jnp.float32),
        "lru_wr": nrm(ks[5], (L, LRU_BLOCKS, LRU_BLOCK_DIM, LRU_BLOCK_DIM), LRU_BLOCK_DIM),
        "lru_br": 0.02 * jax.random.normal(ks[6], (L, LRU_BLOCKS, LRU_BLOCK_DIM), jnp.float32),
        "lru_wi": nrm(ks[7], (L, LRU_BLOCKS, LRU_BLOCK_DIM, LRU_BLOCK_DIM), LRU_BLOCK_DIM),
        "lru_bi": 0.02 * jax.random.normal(ks[8], (L, LRU_BLOCKS, LRU_BLOCK_DIM), jnp.float32),
        "lru_lambda": lru_lambda,
        "w_branch_a": nrm(ks[10], (L, LRU_WIDTH, D_MODEL), LRU_WIDTH),
        "w_branch_b": nrm(ks[11], (L, RET_WIDTH, D_MODEL), RET_WIDTH),
        "w_out": nrm(ks[12], (L, D_MODEL, D_MODEL), D_MODEL),
        "norm2_g": 1.0 + 0.05 * jax.random.normal(ks[13], (L, D_MODEL), jnp.float32),
        "w_ff1": nrm(ks[14], (L, D_MODEL, D_FF), D_MODEL),
        "w_ff2": nrm(ks[15], (L, D_FF, D_MODEL), D_FF),
        "norm_f_g": 1.0 + 0.05 * jax.random.normal(ks[16], (D_MODEL,), jnp.float32),
    }


def reference(x, norm1_g, w_in, conv_w, conv_b, lru_wr, lru_br, lru_wi, lru_bi, lru_lambda,
              w_branch_a, w_branch_b, w_out, norm2_g, w_ff1, w_ff2, norm_f_g):
    bsz, s, _ = x.shape
    pos = jnp.arange(s, dtype=jnp.float32)
    offsets = np.cumsum(np.array(IN_SPLITS))[:-1]
    for l in range(DEPTH):
        h = rmsnorm(x, norm1_g[l])
        proj = h @ w_in[l]
        xa, ga, q, k, v, gr, sa, sb = jnp.split(proj, offsets, axis=-1)
        xa = causal_depthwise_conv(xa, conv_w[l], conv_b[l])
        ya = jax.nn.gelu(ga) * rg_lru(xa, lru_wr[l], lru_br[l], lru_wi[l], lru_bi[l], lru_lambda[l])
        q = rotary(q.reshape(bsz, s, RET_HEADS, RET_HEAD_DIM), pos)
        k = rotary(k.reshape(bsz, s, RET_HEADS, RET_HEAD_DIM), pos) * (RET_HEAD_DIM ** -0.5)
        v = v.reshape(bsz, s, RET_HEADS, RET_HEAD_DIM)
        ret = chunkwise_retention(q, k, v)
        ret = ret * lax.rsqrt(jnp.mean(ret * ret, axis=-1, keepdims=True) + EPS)
        yb = jax.nn.silu(gr) * ret.reshape(bsz, s, RET_WIDTH).astype(x.dtype)
        m = jax.nn.sigmoid(sa) * (ya @ w_branch_a[l]) + jax.nn.sigmoid(sb) * (yb @ w_branch_b[l])
        x = x + m @ w_out[l]
        h2 = rmsnorm(x, norm2_g[l])
        x = x + jnp.square(jax.nn.relu(h2 @ w_ff1[l])) @ w_ff2[l]
    return rmsnorm(x, norm_f_g)
```

```python
import contextlib
import numpy as np
import concourse.bass as bass
import concourse.mybir as mybir
from concourse.bass_utils import run_bass_kernel_spmd

F32 = mybir.dt.float32
BF16 = mybir.dt.bfloat16
AF = mybir.ActivationFunctionType
ALU = mybir.AluOpType
AX = mybir.AxisListType

D = 1024
SEQ = 4096
NB = 4
TOK = 2048
T = 512
NST = TOK // T
EPS = 1e-6
NBLK = 30
PK1 = 200
PK2 = 2560


class _Op:
    __slots__ = ("eng", "fn", "deps", "idx", "is_dma", "sem", "semval", "milestone",
                 "mnum", "epoch", "prewait")


class Sched:
    ENGS = ("pe", "act", "dve", "pool", "sp")
    DMA_KS = {"pe": 8, "act": 8, "dve": 8, "pool": 48, "sp": 8}

    def __init__(self, nc):
        self.nc = nc
        self.ops = {e: [] for e in self.ENGS}
        self.lastw = {}
        self.readers = {}
        self.epoch = 0
        self.dma_n = {e: 0 for e in self.ENGS}
        self.alias = {}
        self.bank_last = {}

    def add_alias(self, a, b):
        self.alias.setdefault(a, []).append(b)
        self.alias.setdefault(b, []).append(a)

    def new_epoch(self):
        self.epoch += 1

    def add(self, eng, fn, reads=(), writes=(), dma=False):
        op = _Op()
        op.eng = eng
        op.fn = fn
        op.is_dma = dma
        op.milestone = False
        op.mnum = 0
        op.epoch = self.epoch
        op.sem = None
        op.semval = 0
        op.prewait = None
        deps = {}
        for k in reads:
            w = self.lastw.get(k)
            if w is not None:
                deps[id(w)] = (w, True)
        for k in list(reads) + list(writes):
            if isinstance(k, tuple) and k and k[0] == "ps":
                bl = self.bank_last.setdefault(k, {})
                for e2, o2 in bl.items():
                    if e2 != eng and id(o2) not in deps:
                        deps[id(o2)] = (o2, False)
                bl[eng] = op
        if self.alias:
            wl = list(writes)
            for k in writes:
                for a in self.alias.get(k, ()):
                    if a not in wl:
                        wl.append(a)
            writes = wl
        for k in writes:
            w = self.lastw.get(k)
            if w is not None and id(w) not in deps:
                deps[id(w)] = (w, False)
            rd = self.readers.get(k)
            if rd:
                for r in rd[0].values():
                    if id(r) not in deps:
                        deps[id(r)] = (r, False)
                for r in rd[1]:
                    if id(r) not in deps:
                        deps[id(r)] = (r, False)
        for k in reads:
            rd = self.readers.get(k)
            if rd is None:
                rd = self.readers[k] = ({}, [])
            if dma:
                rd[1].append(op)
            else:
                rd[0][eng] = op
        for k in writes:
            self.lastw[k] = op
            self.readers[k] = ({}, [])
        fin = []
        for (d, raw) in deps.values():
            if d is op:
                continue
            if (not d.is_dma) and d.eng == eng and not dma:
                if eng == "pe":
                    continue
            fin.append(d)
        op.deps = fin
        lst = self.ops[eng]
        op.idx = len(lst)
        lst.append(op)
        if dma:
            i = self.dma_n[eng]
            self.dma_n[eng] = i + 1
            K = self.DMA_KS[eng]
            op.sem = (eng, i % K)
            op.semval = 16 * (i // K + 1)
            if i >= K:
                op.prewait = (op.sem, 16 * (i // K))
        return op

    def emit(self):
        nc = self.nc
        for e in self.ENGS:
            for op in self.ops[e]:
                for d in op.deps:
                    if not d.is_dma:
                        d.milestone = True
        nep = self.epoch + 1
        for e in self.ENGS:
            cnt = [0] * nep
            for op in self.ops[e]:
                if op.is_dma:
                    continue
                if op.milestone:
                    cnt[op.epoch] += 1
                    op.mnum = cnt[op.epoch]
        stack = contextlib.ExitStack()
        sems = {}
        with stack:
            for e in self.ENGS:
                for p in range(nep):
                    if any((not o.is_dma) and o.milestone and o.epoch == p for o in self.ops[e]):
                        sems[(e, p)] = stack.enter_context(nc.semaphore(f"s_{e}_{p}"))
                if self.dma_n[e]:
                    for k in range(min(self.DMA_KS[e], self.dma_n[e])):
                        sems[("dma", e, k)] = stack.enter_context(nc.semaphore(f"d_{e}_{k}"))
            block = stack.enter_context(nc.Block())
            hooks = {"pe": block.tensor, "act": block.scalar, "dve": block.vector,
                     "pool": block.gpsimd, "sp": block.sync}
            all_dma_last = {}
            for e in self.ENGS:
                for op in self.ops[e]:
                    if op.is_dma:
                        all_dma_last[("dma", e, op.sem[1])] = op.semval

            def mk(e):
                def body(eng):
                    waited = {}

                    def wait(key, val):
                        if waited.get(key, 0) >= val:
                            return
                        waited[key] = val
                        eng.wait_ge(sems[key], val)

                    for op in self.ops[e]:
                        for d in op.deps:
                            if d.is_dma:
                                wait(("dma", d.sem[0], d.sem[1]), d.semval)
                            else:
                                wait((d.eng, d.epoch), d.mnum)
                        if op.prewait is not None:
                            wait(("dma", op.prewait[0][0], op.prewait[0][1]), op.prewait[1])
                        ins = op.fn(eng)
                        if op.is_dma:
                            ins.then_inc(sems[("dma", op.sem[0], op.sem[1])], 16)
                        elif op.milestone:
                            ins.then_inc(sems[(e, op.epoch)], 1)
                    if e == "sp":
                        for key, val in all_dma_last.items():
                            wait(key, val)
                return body

            for e in self.ENGS:
                if self.ops[e] or e == "sp":
                    hooks[e](mk(e))


def build_nc(n_st=2 * NST, dbg=None):
    nc = bass.Bass("TRN2", target_bir_lowering=False)

    def din(name, shape, dt=F32):
        return nc.dram_tensor(name, list(shape), dt, kind="ExternalInput").ap()

    xT_d = din("xT", [D, 2 * TOK])
    w_in = din("w_in", [D, 5120])
    w_a = din("w_a", [512, D])
    w_b = din("w_b", [512, D])
    w_out = din("w_out", [D, D])
    w1 = din("w1", [D, 4096])
    w2 = din("w2", [4096, D])
    wr_d = din("wr", [128, 4, 128])
    wi_d = din("wi", [128, 4, 128])
    pk1_d = din("pk1", [128, PK1])
    pk2_d = din("pk2", [128, PK2])
    cos_d = din("cos2", [128, 32, 64])
    sin_d = din("sin2", [128, 32, 64])
    ident_d = din("ident", [128, 128])
    outT_d = nc.dram_tensor("outT", [D, TOK], F32, kind="ExternalOutput").ap()
    scr = nc.dram_tensor("scr", [22, 128, 4096], BF16, kind="Internal").ap()
    scrw2 = nc.dram_tensor("scrw2", [128, 32, 1024], BF16, kind="Internal").ap()

    S = Sched(nc)

    def sb(name, shape, dt=F32):
        return nc.alloc_sbuf_tensor("sb_" + name, list(shape), dt).ap()

    xT = [sb(f"xT{i}", [128, 8, T]) for i in range(2)]
    hT = sb("hT", [128, 8, T], BF16)
    rstd2 = [sb(f"rstd{i}", [128, T]) for i in range(2)]
    sqb = sb("sqb", [128, 8, T], BF16)
    wring = sb("wring", [128, 4, 4096], BF16)
    xa_ext = [sb(f"xa_ext{i}", [128, T + 3]) for i in range(2)]
    hist = sb("hist", [128, 4, 3])
    xc = [sb(f"xc{i}", [128, T]) for i in range(2)]
    xcb = [sb(f"xcb{i}", [128, T], BF16) for i in range(2)]
    L2 = [sb(f"L2_{i}", [128, T]) for i in range(2)]
    L3 = [sb(f"L3_{i}", [128, T]) for i in range(2)]
    L4 = [sb(f"L4_{i}", [128, T]) for i in range(2)]
    L5 = [sb(f"L5_{i}", [128, T]) for i in range(2)]
    gg = [sb(f"gg{i}", [128, T]) for i in range(2)]
    hstate = sb("hstate", [128, 4])
    yaT = sb("yaT", [128, 4, T], BF16)
    ybT = sb("ybT", [128, 4, T], BF16)
    hidT = sb("hidT", [128, 32, T], BF16)
    hflat = hidT.rearrange("p f t -> p (f t)")

    def ov(f0, nf, dt, key):
        v = hflat[:, f0 * T:(f0 + nf) * T]
        if dt == F32:
            v = v.bitcast(F32)
        for f in range(f0, f0 + nf):
            S.add_alias(key, ("hid", f))
        return v

    q_r = [ov(0 + t, 1, BF16, ("q_r", t)) for t in range(4)]
    k_r = [ov(4 + t, 1, BF16, ("k_r", t)) for t in range(4)]
    kdc = [ov(8 + t, 1, BF16, ("kdc", t)) for t in range(4)]
    v_t = [ov(12 + t, 1, BF16, ("v_t", t)) for t in range(4)]
    sgr = [ov(16 + 2 * t, 2, F32, ("sgr", t)) for t in range(4)]
    rt1 = ov(24, 2, F32, "rt1")
    rt2 = ov(26, 2, F32, "rt2")
    qTs = sb("qTs", [128, 4, 128], BF16)
    qTd = [sb(f"qTd{i}", [128, 4, 128], BF16) for i in range(2)]
    kTs = sb("kTs", [128, 4, 128], BF16)
    scT = [sb(f"scT{i}", [128, 2, 4, 128], BF16) for i in range(2)]
    sqr = ov(28, 2, F32, "sqr")
    tmp2 = ov(30, 2, F32, "tmp2")
    ybt = sb("ybt", [128, 512], BF16)
    ssq = sb("ssq", [128, 8])
    st32 = sb("st32", [128, 4, 128])
    stbf = [sb(f"stbf{i}", [128, 4, 128], BF16) for i in range(2)]
    kvt = sb("kvt", [128, 4, 128])
    siga = [sb(f"siga{i}", [128, T]) for i in range(2)]
    sigb = [sb(f"sigb{i}", [128, T]) for i in range(2)]
    mT = sb("mT", [128, 8, T], BF16)
    rl = [sb(f"rl{i}", [128, T]) for i in range(2)]
    cos_s = [sb(f"cos_s{i}", [128, 4, 64]) for i in range(2)]
    sin_s = [sb(f"sin_s{i}", [128, 4, 64]) for i in range(2)]
    pk1 = sb("pk1", [128, PK1])
    pk2 = sb("pk2", [128, PK2])
    g1, g2, gf = pk1[:, 0:8], pk1[:, 8:16], pk1[:, 16:24]
    convw = pk1[:, 24:40].rearrange("p (j k) -> p j k", j=4)
    convb, br, bi, lam = pk1[:, 40:44], pk1[:, 44:48], pk1[:, 48:52], pk1[:, 52:56]
    flag = pk1[:, 56:57]
    kdec = pk1[:, 64:72]
    kdecN = pk1[:, 72:200].rearrange("p (n h) -> p n h", n=16)
    dtab = pk2[:, 0:1024].rearrange("p (a j n) -> p a j n", a=2, j=4)
    qdec = pk2[:, 1024:1536].rearrange("p (j n) -> p j n", j=4)
    cdt = pk2[:, 1536:2048].rearrange("p (j n) -> p j n", j=4)
    maskbd = pk2[:, 2048:2560].rearrange("p (j n) -> p j n", j=4)
    ident = sb("ident", [128, 128], BF16)
    ones = sb("ones", [128, 128], BF16)
    n8sp = sb("n8sp", [128, 4])
    n16sp = sb("n16sp", [128, 4])
    n4sp = sb("n4sp", [128, 4])
    hbr = sb("hbr", [128, 4])
    hbi = sb("hbi", [128, 4])
    qtrc = sb("qtrc", [128, 1])
    epsc = sb("epsc", [128, 1])
    onec = sb("onec", [128, 1])
    wr32 = rt1.rearrange("p (j n) -> p j n", j=4)
    wi32 = rt2.rearrange("p (j n) -> p j n", j=4)
    wrbd = sb("wrbd", [128, 4, 128], BF16)
    wibd = sb("wibd", [128, 4, 128], BF16)

    banks = [nc.alloc_psum_tensor(f"ps{b}", [128, 512], F32).ap() if b != 3 else None for b in range(8)]
    pT_all = nc.alloc_psum_tensor("ps3", [128, 8, 128], BF16).ap()
    rot = [0]
    ROT = [0, 1, 2, 4, 5]

    def nextbank():
        b = ROT[rot[0] % 5]
        rot[0] += 1
        return b

    mrot = [0]
    MROT = [0, 1, 2, 4, 5, 7]

    def nextbank_m():
        b = MROT[mrot[0] % len(MROT)]
        mrot[0] += 1
        return b

    def bk(b):
        return ("ps", b)

    def dma(eng, out, in_, reads, writes):
        S.add(eng, lambda e, o=out, i=in_: e.dma_start(out=o, in_=i), reads, writes, dma=True)

    def act(out, in_, func, reads, writes, bias=None, scale=None):
        kw = {}
        if bias is not None:
            kw["bias"] = bias
        if scale is not None:
            kw["scale"] = scale
        S.add("act", lambda e, o=out, i=in_, f=func, kw=kw: e.activation(out=o, in_=i, func=f, **kw),
              reads, writes)

    def tt(eng, out, in0, in1, op, reads, writes):
        S.add(eng, lambda e, o=out, a=in0, b=in1, p=op: e.tensor_tensor(out=o, in0=a, in1=b, op=p),
              reads, writes)

    def ts(eng, out, in0, s1, op0, reads, writes, s2=None, op1=None):
        if op1 is None:
            S.add(eng, lambda e, o=out, a=in0, s=s1, p=op0: e.tensor_scalar(out=o, in0=a, scalar1=s, scalar2=None, op0=p),
                  reads, writes)
        else:
            S.add(eng, lambda e, o=out, a=in0, s=s1, p=op0, s_2=s2, p1=op1:
                  e.tensor_scalar(out=o, in0=a, scalar1=s, scalar2=s_2, op0=p, op1=p1), reads, writes)

    def stt(out, in0, scalar, in1, op0, op1, reads, writes):
        S.add("dve", lambda e, o=out, a=in0, s=scalar, b=in1, p0=op0, p1=op1:
              e.scalar_tensor_tensor(out=o, in0=a, scalar=s, in1=b, op0=p0, op1=p1), reads, writes)

    def copy(eng, out, in_, reads, writes):
        if eng == "act":
            S.add("act", lambda e, o=out, i=in_: e.copy(out=o, in_=i), reads, writes)
        else:
            S.add(eng, lambda e, o=out, i=in_: e.tensor_copy(out=o, in_=i), reads, writes)

    def mm_group(out, okey, pairs, reads):
        n = len(pairs)
        for i, pr in enumerate(pairs):
            l, r = pr[0], pr[1]
            rk = list(reads) + (list(pr[2]) if len(pr) > 2 else [])
            S.add("pe", lambda e, o=out, l=l, r=r, st=(i == 0), sp=(i == n - 1):
                  e.matmul(o, lhsT=l, rhs=r, start=st, stop=sp), rk, [okey])

    def mm1(out, okey, l, r, reads, start=True, stop=True):
        S.add("pe", lambda e, o=out, l=l, r=r, st=start, sp=stop: e.matmul(o, lhsT=l, rhs=r, start=st, stop=sp),
              reads, [okey])

    def transp(out, okey, in_, reads):
        S.add("pe", lambda e, o=out, i=in_: e.transpose(o, i, ident), list(reads) + ["ident"], [okey])

    def v3(ap, a):
        return ap.rearrange("p (a b) -> p a b", a=a)

    def blk_src(b):
        dst = scr[b]
        if b < 10:
            return [(v3(dst, 8), w_in.rearrange("(c p) n -> p c n", p=128)[:, :, b * 512:(b + 1) * 512])]
        if b < 12:
            k = b - 10
            d4 = dst.rearrange("p (s c n) -> p s c n", s=2, c=4)
            return [(d4[:, 0], w_a.rearrange("(c p) n -> p c n", p=128)[:, :, k * 512:(k + 1) * 512]),
                    (d4[:, 1], w_b.rearrange("(c p) n -> p c n", p=128)[:, :, k * 512:(k + 1) * 512])]
        if b < 14:
            k = b - 12
            return [(v3(dst, 8), w_out.rearrange("(c p) n -> p c n", p=128)[:, :, k * 512:(k + 1) * 512])]
        if b < 22:
            k = b - 14
            return [(v3(dst, 8), w1.rearrange("(c p) n -> p c n", p=128)[:, :, k * 512:(k + 1) * 512])]
        j = b - 22
        d3 = v3(dst, 32)
        s3 = w2.rearrange("(f p) n -> p f n", p=128)[:, :, j * 128:(j + 1) * 128]
        return [(d3[:, q * 8:(q + 1) * 8, :], s3[:, q * 8:(q + 1) * 8, :]) for q in range(4)]

    dbg = dbg or set()
    cast_rest = [1, 2, 5, 10, 6, 8, 11, 7, 9] + list(range(12, 30))

    def cast_block(b):
        if b >= 22:
            q = b - 22
            dma("pool", scrw2[:, q * 4:(q + 1) * 4, :],
                w2.rearrange("(f p) n -> p f n", p=128)[:, q * 4:(q + 1) * 4, :], [], [("scrw2", q)])
            return
        for (dv, sv) in blk_src(b):
            dma("pool", dv, sv, [], [("scr", b)])

    def cast_quantum():
        if cast_rest:
            cast_block(cast_rest.pop(0))

    for b in (0, 3, 4):
        cast_block(b)

    ring_free = [0, 1, 2, 3]

    def wload(b):
        s = ring_free.pop(0)
        if b >= 22:
            j = b - 22
            dma("sp", wring[:, s, :].rearrange("p (f n) -> p f n", f=32), scrw2[:, :, j * 128:(j + 1) * 128],
                [("scrw2", q) for q in range(8)], [("w", s)])
        else:
            dma("sp", wring[:, s, :], scr[b], [("scr", b)], [("w", s)])
        return wring[:, s, :], ("w", s), s

    def wfree(s):
        ring_free.append(s)

    dma("sp", xT[0], xT_d.rearrange("(c p) t -> p c t", p=128)[:, :, 0:T], [], [("xT", 0, c) for c in range(8)])
    dma("sp", cos_s[0], cos_d[:, 0:4, :], [], [("cos", 0)])
    dma("sp", sin_s[0], sin_d[:, 0:4, :], [], [("sin", 0)])
    dma("sp", pk1, pk1_d, [], ["g1", "g2", "gf", "convw", "convb", "br", "bi", "lam", "flag", "kdec", "kdecN"])
    dma("sp", wr32, wr_d, [], ["rt1"])
    dma("sp", wi32, wi_d, [], ["rt2"])
    dma("sp", pk2, pk2_d, [], ["dtab", "qdec", "cdt", "maskbd"])
    dma("pool", ident, ident_d, [], ["ident"])
    S.add("dve", lambda e: e.memset(ones, 1.0 / 1024.0), [], ["ones"])
    S.add("dve", lambda e: e.memset(epsc, EPS), [], ["epsc"])
    S.add("dve", lambda e: e.memset(onec, 1.0), [], ["onec"])
    S.add("dve", lambda e: e.memset(hist, 0.0), [], ["hist"])
    S.add("dve", lambda e: e.memset(hstate, 0.0), [], ["hstate"])
    S.add("dve", lambda e: e.memset(st32, 0.0), [], ["st32"])
    S.add("dve", lambda e: e.memset(stbf[0], 0.0), [], [("stbf", 0)])
    copy("dve", wrbd, wr32, ["rt1"], ["wrbd"])
    copy("dve", wibd, wi32, ["rt2"], ["wibd"])
    act(n8sp, lam, AF.Exp, ["lam"], ["n8sp"], scale=-1.0)
    act(n8sp, n8sp, AF.Ln, ["n8sp", "onec"], ["n8sp"], bias=onec[:, 0:1], scale=1.0)
    ts("dve", n16sp, n8sp, -16.0, ALU.mult, ["n8sp"], ["n16sp"])
    ts("dve", n4sp, n8sp, -4.0, ALU.mult, ["n8sp"], ["n4sp"])
    ts("dve", n8sp, n8sp, -8.0, ALU.mult, ["n8sp", "n16sp", "n4sp"], ["n8sp"])
    ts("dve", hbr, br, 0.5, ALU.mult, ["br"], ["hbr"])
    ts("dve", hbi, bi, 0.5, ALU.mult, ["bi"], ["hbi"])
    S.add("dve", lambda e: e.memset(qtrc, 0.25), [], ["qtrc"])

    def xkeys(xb):
        return [("xT", xb, c) for c in range(8)]

    def hkeys():
        return [("hT", c) for c in range(8)]

    def load_x(st_idx):
        xb = st_idx % 2
        src = xT_d.rearrange("(c p) t -> p c t", p=128)[:, :, st_idx * T:(st_idx + 1) * T]
        dma("sp", xT[xb], src, [], xkeys(xb))
        cb = st_idx % 2
        dma("sp", cos_s[cb], cos_d[:, st_idx * 4:(st_idx + 1) * 4, :], [], [("cos", cb)])
        dma("sp", sin_s[cb], sin_d[:, st_idx * 4:(st_idx + 1) * 4, :], [], [("sin", cb)])

    def norm_stats_chunk(xb, c, sbank):
        act(sqb[:, c, :], xT[xb][:, c, :], AF.Square, [("xT", xb, c)], [("sq", c)])
        S.add("pe", lambda e, c=c, sbank=sbank: e.matmul(banks[sbank], lhsT=ones, rhs=sqb[:, c, :],
                                                         start=(c == 0), stop=(c == 7)),
              ["ones", ("sq", c)], [bk(sbank)])

    def norm_finish(sbank, w):
        act(rstd2[w], banks[sbank], AF.Ln, [bk(sbank), "epsc"], [("rstd", w)], bias=epsc[:, 0:1], scale=1.0)
        act(rstd2[w], rstd2[w], AF.Exp, [("rstd", w)], [("rstd", w)], scale=-0.5)

    def norm_apply(xb, gcol, gkey, w, to_x, after=None):
        for c in range(8):
            xk = ("xT", xb, c)
            if to_x:
                stt(xT[xb][:, c, :], xT[xb][:, c, :], gcol[:, c:c + 1], rstd2[w], ALU.mult, ALU.mult,
                    [xk, gkey, ("rstd", w)], [xk])
                if after is not None:
                    after(c)
            else:
                stt(hT[:, c, :], xT[xb][:, c, :], gcol[:, c:c + 1], rstd2[w], ALU.mult, ALU.mult,
                    [xk, gkey, ("rstd", w)], [("hT", c)])

    def norm1_stats(st_idx):
        xb = st_idx % 2
        for c in range(8):
            norm_stats_chunk(xb, c, 6)
        norm_finish(6, 1)

    tokbank = [0]

    def tok_unit(nm, t, wb_, kw, main, cb, st_idx, bpair=(5, 7)):
        pb = bpair[tokbank[0] % len(bpair)]
        tokbank[0] += 1
        mm_group(banks[pb], bk(pb), [(hT[:, c, t * 128:(t + 1) * 128], wb_[:, c, :], [("hT", c)]) for c in range(8)],
                 [kw])
        P3 = banks[pb].rearrange("p (h d) -> p h d", h=8)
        if nm in ("q", "k"):
            dst = q_r[t] if nm == "q" else k_r[t]
            dkey = ("q_r", t) if nm == "q" else ("k_r", t)
            cs = cos_s[cb][:, t, :].unsqueeze(1).broadcast_to([128, 8, 64])
            s_lo = sin_s[cb][:, t, 0:32].unsqueeze(1).broadcast_to([128, 8, 32])
            s_hi = sin_s[cb][:, t, 32:64].unsqueeze(1).broadcast_to([128, 8, 32])
            r1 = rt1.rearrange("p (h d) -> p h d", h=8)
            r2 = rt2.rearrange("p (h d) -> p h d", h=8)
            tt("dve", r1, P3, cs, ALU.mult, [bk(pb), ("cos", cb)], ["rt1"])
            tt("dve", r2[:, :, 0:32], P3[:, :, 32:64], s_lo, ALU.mult, [bk(pb), ("sin", cb)], ["rt2"])
            tt("dve", r2[:, :, 32:64], P3[:, :, 0:32], s_hi, ALU.mult, [bk(pb), ("sin", cb)], ["rt2"])
            tt("pool", dst, rt1, rt2, ALU.add, ["rt1", "rt2"], [dkey])
            if nm == "k":
                if main:
                    kd3 = kdec.unsqueeze(2).broadcast_to([128, 8, 64])
                else:
                    kd3 = kdecN[:, st_idx * 4 + t, :].unsqueeze(2).broadcast_to([128, 8, 64])
                tt("pool", kdc[t].rearrange("p (h d) -> p h d", h=8),
                   k_r[t].rearrange("p (h d) -> p h d", h=8), kd3, ALU.mult,
                   [("k_r", t), "kdec", "kdecN"], [("kdc", t)])
        elif nm == "v":
            copy("act", v_t[t], banks[pb], [bk(pb)], [("v_t", t)])
        else:
            act(sgr[t], banks[pb], AF.Silu, [bk(pb)], [("sgr", t)])

    def lru_pieces(main, st_idx):
        W = {}

        def load():
            w_, k_, sl = wload(0)
            W["xa"] = (v3(w_, 8), k_, sl)
            if main:
                w_, k_, sl = wload(1)
                W["ga"] = (v3(w_, 8), k_, sl)

        def A(j):
            jb = j % 2
            wxa, kxa, _ = W["xa"]
            mm_group(banks[0], bk(0), [(wxa[:, c, j * 128:(j + 1) * 128], hT[:, c, :], [("hT", c)]) for c in range(8)],
                     [kxa])
            copy("act", xa_ext[jb][:, 3:T + 3], banks[0], [bk(0)], [("xa_ext", jb)])
            copy("pool", xa_ext[jb][:, 0:3], hist[:, j, :], ["hist"], [("xa_ext", jb)])
            ts("dve", xc[jb], xa_ext[jb][:, 0:T], convw[:, j, 0:1], ALU.mult,
               [("xa_ext", jb), "convw", "convb"], [("xc", jb)], s2=convb[:, j:j + 1], op1=ALU.add)
            for k in range(1, 4):
                stt(xc[jb], xa_ext[jb][:, k:k + T], convw[:, j, k:k + 1], xc[jb], ALU.mult, ALU.add,
                    [("xa_ext", jb), "convw", ("xc", jb)], [("xc", jb)])
            copy("pool", hist[:, j, :], xa_ext[jb][:, T:T + 3], [("xa_ext", jb)], ["hist"])
            copy("dve", xcb[jb], xc[jb], [("xc", jb)], [("xcb", jb)])
            if main:
                wga, kga, _ = W["ga"]
                mm_group(banks[4], bk(4), [(wga[:, c, j * 128:(j + 1) * 128], hT[:, c, :], [("hT", c)]) for c in range(8)],
                         [kga])
                act(gg[jb], banks[4], AF.Gelu_apprx_tanh, [bk(4)], [("gg", jb)])
            if j == 3:
                wfree(W["xa"][2])
                if main:
                    wfree(W["ga"][2])

        def B(j):
            jb = j % 2
            mm1(banks[2], bk(2), wrbd[:, j, :], xcb[jb], ["wrbd", ("xcb", jb)])
            mm1(banks[6], bk(6), wibd[:, j, :], xcb[jb], ["wibd", ("xcb", jb)])
            act(L2[jb], banks[2], AF.Tanh, [bk(2), "hbr"], [("L2", jb)], bias=hbr[:, j:j + 1], scale=0.5)
            act(L3[jb], banks[6], AF.Tanh, [bk(6), "hbi"], [("L3", jb)], bias=hbi[:, j:j + 1], scale=0.5)
            act(L4[jb], L2[jb], AF.Exp, [("L2", jb), "n4sp"], [("L4", jb)], bias=n4sp[:, j:j + 1],
                scale=n4sp[:, j:j + 1])
            act(L2[jb], L2[jb], AF.Exp, [("L2", jb), "n8sp"], [("L2", jb)], bias=n8sp[:, j:j + 1],
                scale=n8sp[:, j:j + 1])
            act(L2[jb], L2[jb], AF.Sqrt, [("L2", jb), "qtrc"], [("L2", jb)], bias=qtrc[:, 0:1], scale=-0.25)
            stt(L3[jb], L3[jb], 1.0, xc[jb], ALU.add, ALU.mult, [("L3", jb), ("xc", jb)], [("L3", jb)])
            tt("pool", L3[jb], L3[jb], L2[jb], ALU.mult, [("L3", jb), ("L2", jb)], [("L3", jb)])
            S.add("dve", lambda e, jb=jb, j=j: e.tensor_tensor_scan(
                out=L5[jb], data0=L4[jb], data1=L3[jb], initial=hstate[:, j:j + 1], op0=ALU.mult, op1=ALU.add),
                [("L4", jb), ("L3", jb), "hstate"], [("L5", jb)])
            copy("pool", hstate[:, j:j + 1], L5[jb][:, T - 1:T], [("L5", jb)], ["hstate"])
            if main:
                tt("pool", yaT[:, j, :], gg[jb], L5[jb], ALU.mult, [("gg", jb), ("L5", jb)], ["yaT"])
                if st_idx == NST:
                    cast_quantum()

        return load, A, B

    def tok_blocks(names):
        bid = {"q": 2, "k": 3, "v": 4, "gr": 5}
        out = []
        for nm in names:
            w_, k_, sl = wload(bid[nm])
            out.append((nm, v3(w_, 8), k_, sl))
        return out

    def lru_standalone(main, st_idx, cb):
        load, A, B = lru_pieces(main, st_idx)
        load()
        fw = tok_blocks(("q", "v") if main else ("k", "v"))
        for j in range(5):
            if j < 4:
                A(j)
                for (nm, w_, k_, sl) in fw:
                    tok_unit(nm, j, w_, k_, main, cb, st_idx, bpair=(5, 7) if main else (5, 7, 4))
            if j >= 1:
                B(j - 1)
        for (nm, w_, k_, sl) in fw:
            wfree(sl)
        if main:
            rest = tok_blocks(("k", "gr"))
            for t in range(4):
                for (nm, w_, k_, sl) in rest:
                    tok_unit(nm, t, w_, k_, main, cb, st_idx)
            for (nm, w_, k_, sl) in rest:
                wfree(sl)
        else:
            cast_quantum()
            cast_quantum()
            cast_quantum()

    PEND = []

    def tok_phase(st_idx, cb):
        blks = tok_blocks(("q", "v", "k", "gr"))
        for t in range(4):
            for (nm, w_, k_, sl) in blks:
                tok_unit(nm, t, w_, k_, True, cb, st_idx, bpair=(0, 1, 2))
            if t == 0 and PEND:
                PEND.pop(0)()
            if t >= 1:
                ret_A(t - 1, (t - 1) % 2)
            if t >= 2:
                ret_B(t - 2, (t - 2) % 2)
        for (nm, w_, k_, sl) in blks:
            wfree(sl)
        ret_A(3, 1)
        ret_B(2, 0)
        ret_B(3, 1)

    stv = [0]

    def kv_update(t):
        cur = stv[0] % 2
        nxt = 1 - cur
        kvp = banks[7].rearrange("p (j n) -> p j n", j=4)
        tt("pool", st32, st32, cdt, ALU.mult, ["st32", "cdt"], ["st32"])
        for j in range(4):
            mm1(kvp[:, j, :], bk(7), kdc[t][:, j * 128:(j + 1) * 128], v_t[t][:, j * 128:(j + 1) * 128],
                [("kdc", t), ("v_t", t)])
        tt("dve", kvt, kvp, maskbd, ALU.mult, [bk(7), "maskbd"], ["kvt"])
        tt("dve", st32, st32, kvt, ALU.add, ["st32", "kvt"], ["st32"])
        copy("act", stbf[nxt], st32, ["st32"], [("stbf", nxt)])
        stv[0] += 1

    def kv_prepass():
        kvp = banks[7].rearrange("p (j n) -> p j n", j=4)
        for j in range(4):
            for t in range(4):
                mm1(kvp[:, j, :], bk(7), kdc[t][:, j * 128:(j + 1) * 128], v_t[t][:, j * 128:(j + 1) * 128],
                    [("kdc", t), ("v_t", t)], start=(t == 0), stop=(t == 3))
        tt("dve", kvt, kvp, maskbd, ALU.mult, [bk(7), "maskbd"], ["kvt"])
        tt("dve", st32, st32, kvt, ALU.add, ["st32", "kvt"], ["st32"])

    def ret_A(t, sci):
        pT = pT_all
        for j in range(4):
            transp(pT[:, j, :], bk(3), q_r[t][:, j * 128:(j + 1) * 128], [("q_r", t)])
        for j in range(4):
            transp(pT[:, 4 + j, :], bk(3), k_r[t][:, j * 128:(j + 1) * 128], [("k_r", t)])
        copy("act", qTs, pT[:, 0:4, :], [bk(3)], ["qTs"])
        copy("act", kTs, pT[:, 4:8, :], [bk(3)], ["kTs"])
        tt("dve", qTd[sci], pT[:, 0:4, :], qdec, ALU.mult, [bk(3), "qdec"], [("qTd", sci)])
        for h in range(8):
            par, j = h % 2, h // 2
            o = par * 64
            mm1(banks[4 + par][:, j * 128:(j + 1) * 128], bk(4 + par), kTs[o:o + 64, j, :], qTs[o:o + 64, j, :],
                ["kTs", "qTs"])
        sc = scT[sci]
        for par in range(2):
            tt("dve", sc[:, par].rearrange("p j n -> p (j n)"), banks[4 + par],
               dtab[:, par].rearrange("p j n -> p (j n)"), ALU.mult, [bk(4 + par), "dtab"], [("scT", sci)])

    def ret_B(t, sci):
        cur = stv[0] % 2
        pT = pT_all
        sc = scT[sci]
        for j in range(4):
            mm1(banks[6][:, j * 128:(j + 1) * 128], bk(6), qTd[sci][:, j, :], stbf[cur][:, j, :],
                [("qTd", sci), ("stbf", cur)], start=True, stop=False)
            for par in range(2):
                h = 2 * j + par
                mm1(banks[6][:, h * 64:(h + 1) * 64], bk(6), sc[:, par, j, :], v_t[t][:, h * 64:(h + 1) * 64],
                    [("scT", sci), ("v_t", t)], start=False, stop=(par == 1))
        kv_update(t)
        act(sqr, banks[6], AF.Square, [bk(6)], ["sqr"])
        S.add("dve", lambda e: e.tensor_reduce(out=ssq, in_=sqr.rearrange("p (h d) -> p h d", h=8),
                                               op=ALU.add, axis=AX.X), ["sqr"], ["ssq"])
        act(ssq, ssq, AF.Ln, ["ssq", "epsc"], ["ssq"], bias=epsc[:, 0:1], scale=1.0 / 64.0)
        act(ssq, ssq, AF.Exp, ["ssq"], ["ssq"], scale=-0.5)
        tt("dve", tmp2.rearrange("p (h d) -> p h d", h=8), banks[6].rearrange("p (h d) -> p h d", h=8),
           ssq.unsqueeze(2).broadcast_to([128, 8, 64]), ALU.mult, [bk(6), "ssq"], ["tmp2"])
        tt("pool", ybt, tmp2, sgr[t], ALU.mult, ["tmp2", ("sgr", t)], ["ybt"])
        for j in range(4):
            transp(pT[:, j, :], bk(3), ybt[:, j * 128:(j + 1) * 128], ["ybt"])
        copy("act", ybT[:, :, t * 128:(t + 1) * 128], pT[:, 0:4, :], [bk(3)], ["ybT"])

    def retention():
        for f, t, b in ((ret_A, 0, 0), (ret_A, 1, 1), (ret_B, 0, 0), (ret_A, 2, 0), (ret_B, 1, 1), (ret_A, 3, 1),
                        (ret_B, 2, 0), (ret_B, 3, 1)):
            f(t, b)
            cast_quantum()

    def merge(xb):
        for half in range(2):
            wab, kab, sl_ab = wload(10 + half)
            wab = wab.rearrange("p (s c n) -> p s c n", s=2, c=4)
            wsa, ksa, sl_sa = wload(6 + half)
            wsa = v3(wsa, 8)
            wsb, ksb, sl_sb = wload(8 + half)
            wsb = v3(wsb, 8)
            for jj in range(4):
                j = half * 4 + jj
                jb = j % 2
                cs = slice(jj * 128, (jj + 1) * 128)
                bsa, bsb, bpa, bpb = nextbank_m(), nextbank_m(), nextbank_m(), nextbank_m()
                mm_group(banks[bsa], bk(bsa), [(wsa[:, c, cs], hT[:, c, :], [("hT", c)]) for c in range(8)], [ksa])
                mm_group(banks[bsb], bk(bsb), [(wsb[:, c, cs], hT[:, c, :], [("hT", c)]) for c in range(8)], [ksb])
                mm_group(banks[bpa], bk(bpa), [(wab[:, 0, c, cs], yaT[:, c, :]) for c in range(4)], [kab, "yaT"])
                mm_group(banks[bpb], bk(bpb), [(wab[:, 1, c, cs], ybT[:, c, :]) for c in range(4)], [kab, "ybT"])
                act(siga[jb], banks[bsa], AF.Sigmoid, [bk(bsa)], [("siga", jb)])
                act(sigb[jb], banks[bsb], AF.Sigmoid, [bk(bsb)], [("sigb", jb)])
                tt("dve", siga[jb], banks[bpa], siga[jb], ALU.mult, [bk(bpa), ("siga", jb)], [("siga", jb)])
                tt("dve", sigb[jb], banks[bpb], sigb[jb], ALU.mult, [bk(bpb), ("sigb", jb)], [("sigb", jb)])
                tt("pool", mT[:, j, :], siga[jb], sigb[jb], ALU.add, [("siga", jb), ("sigb", jb)], [("mT", j)])
            wfree(sl_ab)
            wfree(sl_sa)
            wfree(sl_sb)
            cast_quantum()
            cast_quantum()
        for half in range(2):
            wo, ko, sl_o = wload(12 + half)
            wo = v3(wo, 8)
            for jj in range(4):
                j = half * 4 + jj
                pb = nextbank()
                mm_group(banks[pb], bk(pb), [(wo[:, c, jj * 128:(jj + 1) * 128], mT[:, c, :], [("mT", c)]) for c in range(8)],
                         [ko])
                tt("dve", xT[xb][:, j, :], banks[pb], xT[xb][:, j, :], ALU.add, [bk(pb), ("xT", xb, j)],
                   [("xT", xb, j)])
                if j >= 1:
                    norm_stats_chunk(xb, j - 1, 7)
            wfree(sl_o)
        norm_stats_chunk(xb, 7, 7)
        norm_finish(7, 0)

    LRU_SCHED = [[("A", 0)], [("A", 1)], [("B", 0)], [("A", 2)], [("B", 1)], [("A", 3)], [("B", 2)], [("B", 3)]]

    def ffn(xb, st_idx):
        n = 0
        for b in range(8):
            wv, kw, sl = wload(14 + b)
            wv = v3(wv, 8)
            for i in range(4):
                pb = nextbank()
                rb = n % 2
                n += 1
                mm_group(banks[pb], bk(pb), [(wv[:, c, i * 128:(i + 1) * 128], hT[:, c, :], [("hT", c)]) for c in range(8)],
                         [kw])
                act(rl[rb], banks[pb], AF.Relu, [bk(pb)], [("rl", rb)])
                tt("pool", hidT[:, b * 4 + i, :], rl[rb], rl[rb], ALU.mult, [("rl", rb)], [("hid", b * 4 + i)])
            wfree(sl)
            if b == 4 and st_idx + 1 < n_st:
                norm1_stats(st_idx + 1)
        overlap = st_idx + 1 < n_st
        if overlap:
            norm_apply((st_idx + 1) % 2, g1, "g1", 1, to_x=False)
            load, A, B = lru_pieces(True, st_idx + 1)
        for j in range(8):
            wv, kw, sl = wload(22 + j)
            wv = v3(wv, 32)
            pb = (1, 5)[j % 2]
            mm_group(banks[pb], bk(pb), [(wv[:, f, :], hidT[:, f, :], [("hid", f)]) for f in range(32)], [kw])
            wfree(sl)
            tt("dve", xT[xb][:, j, :], banks[pb], xT[xb][:, j, :], ALU.add, [bk(pb), ("xT", xb, j)],
               [("xT", xb, j)])
            if j >= 1:
                norm_stats_chunk(xb, j - 1, 7)
            if overlap:
                if j == 0:
                    load()
                for kind, jj in LRU_SCHED[j]:
                    (A if kind == "A" else B)(jj)
        norm_stats_chunk(xb, 7, 7)
        norm_finish(7, 0)

    sci = [0]
    norm1_stats(0)
    for st_idx in range(n_st):
        main = st_idx >= NST
        xb = st_idx % 2
        cb = st_idx % 2
        if st_idx == NST:
            while cast_rest and cast_rest[0] < 14:
                cast_quantum()
            ts("dve", hstate, hstate, flag[:, 0:1], ALU.mult, ["hstate", "flag"], ["hstate"])
        if st_idx == 0:
            norm_apply(xb, g1, "g1", 1, to_x=False)
        if st_idx <= NST:
            lru_standalone(main, st_idx, cb)
        else:
            tok_phase(st_idx, cb)
        if st_idx + 1 < n_st:
            load_x(st_idx + 1)
        if main:
            if st_idx == NST:
                retention()
            merge(xb)
            norm_apply(xb, g2, "g2", 0, to_x=False)
            while cast_rest:
                cast_quantum()
            ffn(xb, st_idx)
            m = st_idx - NST
            dst = outT_d.rearrange("(c p) t -> p c t", p=128)[:, :, m * T:(m + 1) * T]
            if st_idx == n_st - 1:
                norm_apply(xb, gf, "gf", 0, to_x=True,
                           after=lambda c: dma("act", dst[:, c, :], xT[xb][:, c, :], [("xT", xb, c)], [("out", m, c)]))
            else:
                norm_apply(xb, gf, "gf", 0, to_x=True)
                PEND.append(lambda dst=dst, xb=xb, m=m: dma("act", dst, xT[xb], xkeys(xb), [("out", m)]))
        else:
            if st_idx + 1 < n_st:
                norm1_stats(st_idx + 1)
                norm_apply((st_idx + 1) % 2, g1, "g1", 1, to_x=False)
            kv_prepass()
            if st_idx == NST - 1:
                copy("act", stbf[stv[0] % 2], st32, ["st32"], [("stbf", stv[0] % 2)])
        S.new_epoch()
    S.emit()
    return nc


def _const_tables():
    nh, dh, C = 8, 64, 128
    log_g = np.log1p(-np.exp2(-5.0 - np.arange(nh, dtype=np.float64)))
    pos = np.arange(C, dtype=np.float64)
    rel = pos[None, :] - pos[:, None]
    dtab = np.zeros((C, 2, 4, C), np.float64)
    for h in range(nh):
        dtab[:, h % 2, h // 2, :] = np.where(rel >= 0, np.exp(log_g[h] * np.maximum(rel, 0.0)), 0.0) * 0.125
    qdec = np.zeros((128, 4, C), np.float64)
    cdt = np.zeros((128, 4, 128), np.float64)
    mask = np.zeros((128, 4, 128), np.float64)
    for p in range(128):
        for j in range(4):
            h = 2 * j + p // 64
            qdec[p, j, :] = np.exp(log_g[h] * (pos + 1.0))
            cdt[p, j, :] = np.exp(log_g[h] * C)
            mask[p, j, (p // 64) * 64:(p // 64) * 64 + 64] = 1.0
    kdec = np.zeros((C, nh), np.float64)
    for h in range(nh):
        kdec[:, h] = np.exp(log_g[h] * (C - 1.0 - pos)) * 0.125
    kdecN = np.zeros((C, 16, nh), np.float64)
    for n in range(16):
        for h in range(nh):
            kdecN[:, n, h] = np.exp(log_g[h] * (C - 1.0 - pos + C * (15 - n))) * 0.125
    ident = np.eye(128, dtype=np.float32)
    return (dtab.astype(np.float32), qdec.astype(np.float32), kdec.astype(np.float32),
            cdt.astype(np.float32), mask.astype(np.float32), ident, kdecN.astype(np.float32))


def _rope_tables(pos0):
    half = 32
    inv = 10000.0 ** (-np.arange(half, dtype=np.float64) / half)
    p = np.arange(128)[:, None] + np.arange(32)[None, :] * 128 + pos0
    ang = p.astype(np.float64)[:, :, None] * inv[None, None, :]
    c, s = np.cos(ang), np.sin(ang)
    cos2 = np.concatenate([c, c], axis=-1).astype(np.float32)
    sin2 = np.concatenate([-s, s], axis=-1).astype(np.float32)
    return np.ascontiguousarray(cos2), np.ascontiguousarray(sin2)


def kernel(x, norm1_g, w_in, conv_w, conv_b, lru_wr, lru_br, lru_wi, lru_bi, lru_lambda,
           w_branch_a, w_branch_b, w_out, norm2_g, w_ff1, w_ff2, norm_f_g):
    f32 = np.float32
    x = np.asarray(x, f32)

    def pc(v, n):
        return np.ascontiguousarray(np.asarray(v, f32).reshape(n, 128).T)

    dtab, qdec, kdec, cdt, mask, ident, kdecN = _const_tables()
    shared = {
        "w_in": np.ascontiguousarray(np.asarray(w_in, f32)[0]),
        "w_a": np.ascontiguousarray(np.asarray(w_branch_a, f32)[0]),
        "w_b": np.ascontiguousarray(np.asarray(w_branch_b, f32)[0]),
        "w_out": np.ascontiguousarray(np.asarray(w_out, f32)[0]),
        "w1": np.ascontiguousarray(np.asarray(w_ff1, f32)[0]),
        "w2": np.ascontiguousarray(np.asarray(w_ff2, f32)[0]),
    }
    wr_bd = np.zeros((128, 4, 128), f32)
    wi_bd = np.zeros((128, 4, 128), f32)
    wr0, wi0 = np.asarray(lru_wr, f32)[0], np.asarray(lru_wi, f32)[0]
    for blk in range(8):
        j, o = blk // 2, (blk % 2) * 64
        wr_bd[o:o + 64, j, o:o + 64] = wr0[blk]
        wi_bd[o:o + 64, j, o:o + 64] = wi0[blk]
    shared["wr"] = wr_bd
    shared["wi"] = wi_bd
    shared["ident"] = ident
    pk2 = np.concatenate([dtab.reshape(128, -1), qdec.reshape(128, -1), cdt.reshape(128, -1),
                          mask.reshape(128, -1)], axis=1).astype(f32)
    shared["pk2"] = np.ascontiguousarray(pk2)

    def pack1(flagval):
        pk = np.zeros((128, PK1), f32)
        pk[:, 0:8] = pc(np.asarray(norm1_g)[0], 8)
        pk[:, 8:16] = pc(np.asarray(norm2_g)[0], 8)
        pk[:, 16:24] = pc(norm_f_g, 8)
        pk[:, 24:40] = np.asarray(conv_w, f32)[0].reshape(4, 4, 128).transpose(2, 1, 0).reshape(128, 16)
        pk[:, 40:44] = pc(np.asarray(conv_b)[0], 4)
        pk[:, 44:48] = pc(np.asarray(lru_br)[0].reshape(-1), 4)
        pk[:, 48:52] = pc(np.asarray(lru_bi)[0].reshape(-1), 4)
        pk[:, 52:56] = pc(np.asarray(lru_lambda)[0], 4)
        pk[:, 56] = flagval
        pk[:, 64:72] = kdec
        pk[:, 72:200] = kdecN.reshape(128, 128)
        return pk

    in_maps = []
    for c in range(8):
        b, half = c // 2, c % 2
        xt = np.zeros((D, 2 * TOK), f32)
        if half == 1:
            xt[:, :TOK] = x[b, :TOK].T
        xt[:, TOK:] = x[b, half * TOK:(half + 1) * TOK].T
        cos2, sin2 = _rope_tables(0 if half == 1 else -TOK)
        m = dict(shared)
        m["xT"] = xt
        m["pk1"] = pack1(float(half))
        m["cos2"] = cos2
        m["sin2"] = sin2
        in_maps.append(m)
    nc = build_nc()
    res = run_bass_kernel_spmd(nc, in_maps, core_ids=list(range(8)))
    out = np.empty((NB, SEQ, D), f32)
    for c in range(8):
        b, half = c // 2, c % 2
        out[b, half * TOK:(half + 1) * TOK, :] = np.asarray(res.results[c]["outT"], f32).T
    return out
```

```python
import contextlib
import numpy as np
import concourse.bass as bass
import concourse.mybir as mybir
from concourse.bass_utils import run_bass_kernel_spmd

F32 = mybir.dt.float32
BF16 = mybir.dt.bfloat16
AF = mybir.ActivationFunctionType
ALU = mybir.AluOpType
AX = mybir.AxisListType

D = 1024
SEQ = 4096
NB = 4
TOK = 2048
T = 512
NST = TOK // T
EPS = 1e-6
NBLK = 30
PK1 = 200
PK2 = 2560


class _Op:
    __slots__ = ("eng", "fn", "deps", "idx", "is_dma", "sem", "semval", "milestone",
                 "mnum", "epoch", "prewait")


class Sched:
    ENGS = ("pe", "act", "dve", "pool", "sp")
    DMA_KS = {"pe": 8, "act": 8, "dve": 8, "pool": 48, "sp": 8}

    def __init__(self, nc):
        self.nc = nc
        self.ops = {e: [] for e in self.ENGS}
        self.lastw = {}
        self.readers = {}
        self.epoch = 0
        self.dma_n = {e: 0 for e in self.ENGS}
        self.alias = {}
        self.bank_last = {}

    def add_alias(self, a, b):
        self.alias.setdefault(a, []).append(b)
        self.alias.setdefault(b, []).append(a)

    def new_epoch(self):
        self.epoch += 1

    def add(self, eng, fn, reads=(), writes=(), dma=False):
        op = _Op()
        op.eng = eng
        op.fn = fn
        op.is_dma = dma
        op.milestone = False
        op.mnum = 0
        op.epoch = self.epoch
        op.sem = None
        op.semval = 0
        op.prewait = None
        deps = {}
        for k in reads:
            w = self.lastw.get(k)
            if w is not None:
                deps[id(w)] = (w, True)
        for k in list(reads) + list(writes):
            if isinstance(k, tuple) and k and k[0] == "ps":
                bl = self.bank_last.setdefault(k, {})
                for e2, o2 in bl.items():
                    if e2 != eng and id(o2) not in deps:
                        deps[id(o2)] = (o2, False)
                bl[eng] = op
        if self.alias:
            wl = list(writes)
            for k in writes:
                for a in self.alias.get(k, ()):
                    if a not in wl:
                        wl.append(a)
            writes = wl
        for k in writes:
            w = self.lastw.get(k)
            if w is not None and id(w) not in deps:
                deps[id(w)] = (w, False)
            rd = self.readers.get(k)
            if rd:
                for r in rd[0].values():
                    if id(r) not in deps:
                        deps[id(r)] = (r, False)
                for r in rd[1]:
                    if id(r) not in deps:
                        deps[id(r)] = (r, False)
        for k in reads:
            rd = self.readers.get(k)
            if rd is None:
                rd = self.readers[k] = ({}, [])
            if dma:
                rd[1].append(op)
            else:
                rd[0][eng] = op
        for k in writes:
            self.lastw[k] = op
            self.readers[k] = ({}, [])
        fin = []
        for (d, raw) in deps.values():
            if d is op:
                continue
            if (not d.is_dma) and d.eng == eng and not dma:
                if eng == "pe":
                    continue
            fin.append(d)
        op.deps = fin
        lst = self.ops[eng]
        op.idx = len(lst)
        lst.append(op)
        if dma:
            i = self.dma_n[eng]
            self.dma_n[eng] = i + 1
            K = self.DMA_KS[eng]
            op.sem = (eng, i % K)
            op.semval = 16 * (i // K + 1)
            if i >= K:
                op.prewait = (op.sem, 16 * (i // K))
        return op

    def emit(self):
        nc = self.nc
        for e in self.ENGS:
            for op in self.ops[e]:
                for d in op.deps:
                    if not d.is_dma:
                        d.milestone = True
        nep = self.epoch + 1
        for e in self.ENGS:
            cnt = [0] * nep
            for op in self.ops[e]:
                if op.is_dma:
                    continue
                if op.milestone:
                    cnt[op.epoch] += 1
                    op.mnum = cnt[op.epoch]
        stack = contextlib.ExitStack()
        sems = {}
        with stack:
            for e in self.ENGS:
                for p in range(nep):
                    if any((not o.is_dma) and o.milestone and o.epoch == p for o in self.ops[e]):
                        sems[(e, p)] = stack.enter_context(nc.semaphore(f"s_{e}_{p}"))
                if self.dma_n[e]:
                    for k in range(min(self.DMA_KS[e], self.dma_n[e])):
                        sems[("dma", e, k)] = stack.enter_context(nc.semaphore(f"d_{e}_{k}"))
            block = stack.enter_context(nc.Block())
            hooks = {"pe": block.tensor, "act": block.scalar, "dve": block.vector,
                     "pool": block.gpsimd, "sp": block.sync}
            all_dma_last = {}
            for e in self.ENGS:
                for op in self.ops[e]:
                    if op.is_dma:
                        all_dma_last[("dma", e, op.sem[1])] = op.semval

            def mk(e):
                def body(eng):
                    waited = {}

                    def wait(key, val):
                        if waited.get(key, 0) >= val:
                            return
                        waited[key] = val
                        eng.wait_ge(sems[key], val)

                    for op in self.ops[e]:
                        for d in op.deps:
                            if d.is_dma:
                                wait(("dma", d.sem[0], d.sem[1]), d.semval)
                            else:
                                wait((d.eng, d.epoch), d.mnum)
                        if op.prewait is not None:
                            wait(("dma", op.prewait[0][0], op.prewait[0][1]), op.prewait[1])
                        ins = op.fn(eng)
                        if op.is_dma:
                            ins.then_inc(sems[("dma", op.sem[0], op.sem[1])], 16)
                        elif op.milestone:
                            ins.then_inc(sems[(e, op.epoch)], 1)
                    if e == "sp":
                        for key, val in all_dma_last.items():
                            wait(key, val)
                return body

            for e in self.ENGS:
                if self.ops[e] or e == "sp":
                    hooks[e](mk(e))


def build_nc(n_st=2 * NST, dbg=None):
    nc = bass.Bass("TRN2", target_bir_lowering=False)

    def din(name, shape, dt=F32):
        return nc.dram_tensor(name, list(shape), dt, kind="ExternalInput").ap()

    xT_d = din("xT", [D, 2 * TOK])
    w_in = din("w_in", [D, 5120])
    w_a = din("w_a", [512, D])
    w_b = din("w_b", [512, D])
    w_out = din("w_out", [D, D])
    w1 = din("w1", [D, 4096])
    w2 = din("w2", [4096, D])
    wr_d = din("wr", [128, 4, 128])
    wi_d = din("wi", [128, 4, 128])
    pk1_d = din("pk1", [128, PK1])
    pk2_d = din("pk2", [128, PK2])
    cos_d = din("cos2", [128, 32, 64])
    sin_d = din("sin2", [128, 32, 64])
    ident_d = din("ident", [128, 128])
    outT_d = nc.dram_tensor("outT", [D, TOK], F32, kind="ExternalOutput").ap()
    scr = nc.dram_tensor("scr", [22, 128, 4096], BF16, kind="Internal").ap()
    scrw2 = nc.dram_tensor("scrw2", [128, 32, 1024], BF16, kind="Internal").ap()

    S = Sched(nc)

    def sb(name, shape, dt=F32):
        return nc.alloc_sbuf_tensor("sb_" + name, list(shape), dt).ap()

    xT = [sb(f"xT{i}", [128, 8, T]) for i in range(2)]
    hT = sb("hT", [128, 8, T], BF16)
    rstd2 = [sb(f"rstd{i}", [128, T]) for i in range(2)]
    sqb = sb("sqb", [128, 8, T], BF16)
    wring = sb("wring", [128, 4, 4096], BF16)
    xa_ext = [sb(f"xa_ext{i}", [128, T + 3]) for i in range(2)]
    hist = sb("hist", [128, 4, 3])
    xc = [sb(f"xc{i}", [128, T]) for i in range(2)]
    xcb = [sb(f"xcb{i}", [128, T], BF16) for i in range(2)]
    L2 = [sb(f"L2_{i}", [128, T]) for i in range(2)]
    L3 = [sb(f"L3_{i}", [128, T]) for i in range(2)]
    L4 = [sb(f"L4_{i}", [128, T]) for i in range(2)]
    L5 = [sb(f"L5_{i}", [128, T]) for i in range(2)]
    gg = [sb(f"gg{i}", [128, T]) for i in range(2)]
    hstate = sb("hstate", [128, 4])
    yaT = sb("yaT", [128, 4, T], BF16)
    ybT = sb("ybT", [128, 4, T], BF16)
    hidT = sb("hidT", [128, 32, T], BF16)
    hflat = hidT.rearrange("p f t -> p (f t)")

    def ov(f0, nf, dt, key):
        v = hflat[:, f0 * T:(f0 + nf) * T]
        if dt == F32:
            v = v.bitcast(F32)
        for f in range(f0, f0 + nf):
            S.add_alias(key, ("hid", f))
        return v

    q_r = [ov(0 + t, 1, BF16, ("q_r", t)) for t in range(4)]
    k_r = [ov(4 + t, 1, BF16, ("k_r", t)) for t in range(4)]
    kdc = [ov(8 + t, 1, BF16, ("kdc", t)) for t in range(4)]
    v_t = [ov(12 + t, 1, BF16, ("v_t", t)) for t in range(4)]
    sgr = [ov(16 + 2 * t, 2, F32, ("sgr", t)) for t in range(4)]
    rt1 = ov(24, 2, F32, "rt1")
    rt2 = ov(26, 2, F32, "rt2")
    qTs = sb("qTs", [128, 4, 128], BF16)
    qTd = [sb(f"qTd{i}", [128, 4, 128], BF16) for i in range(2)]
    kTs = sb("kTs", [128, 4, 128], BF16)
    scT = [sb(f"scT{i}", [128, 2, 4, 128], BF16) for i in range(2)]
    sqr = ov(28, 2, F32, "sqr")
    tmp2 = ov(30, 2, F32, "tmp2")
    ybt = sb("ybt", [128, 512], BF16)
    ssq = sb("ssq", [128, 8])
    st32 = sb("st32", [128, 4, 128])
    stbf = [sb(f"stbf{i}", [128, 4, 128], BF16) for i in range(2)]
    kvt = sb("kvt", [128, 4, 128])
    siga = [sb(f"siga{i}", [128, T]) for i in range(2)]
    sigb = [sb(f"sigb{i}", [128, T]) for i in range(2)]
    mT = sb("mT", [128, 8, T], BF16)
    rl = [sb(f"rl{i}", [128, T]) for i in range(2)]
    cos_s = [sb(f"cos_s{i}", [128, 4, 64]) for i in range(2)]
    sin_s = [sb(f"sin_s{i}", [128, 4, 64]) for i in range(2)]
    pk1 = sb("pk1", [128, PK1])
    pk2 = sb("pk2", [128, PK2])
    g1, g2, gf = pk1[:, 0:8], pk1[:, 8:16], pk1[:, 16:24]
    convw = pk1[:, 24:40].rearrange("p (j k) -> p j k", j=4)
    convb, br, bi, lam = pk1[:, 40:44], pk1[:, 44:48], pk1[:, 48:52], pk1[:, 52:56]
    flag = pk1[:, 56:57]
    kdec = pk1[:, 64:72]
    kdecN = pk1[:, 72:200].rearrange("p (n h) -> p n h", n=16)
    dtab = pk2[:, 0:1024].rearrange("p (a j n) -> p a j n", a=2, j=4)
    qdec = pk2[:, 1024:1536].rearrange("p (j n) -> p j n", j=4)
    cdt = pk2[:, 1536:2048].rearrange("p (j n) -> p j n", j=4)
    maskbd = pk2[:, 2048:2560].rearrange("p (j n) -> p j n", j=4)
    ident = sb("ident", [128, 128], BF16)
    ones = sb("ones", [128, 128], BF16)
    n8sp = sb("n8sp", [128, 4])
    n16sp = sb("n16sp", [128, 4])
    n4sp = sb("n4sp", [128, 4])
    hbr = sb("hbr", [128, 4])
    hbi = sb("hbi", [128, 4])
    qtrc = sb("qtrc", [128, 1])
    epsc = sb("epsc", [128, 1])
    onec = sb("onec", [128, 1])
    wr32 = rt1.rearrange("p (j n) -> p j n", j=4)
    wi32 = rt2.rearrange("p (j n) -> p j n", j=4)
    wrbd = sb("wrbd", [128, 4, 128], BF16)
    wibd = sb("wibd", [128, 4, 128], BF16)

    banks = [nc.alloc_psum_tensor(f"ps{b}", [128, 512], F32).ap() if b != 3 else None for b in range(8)]
    pT_all = nc.alloc_psum_tensor("ps3", [128, 8, 128], BF16).ap()
    rot = [0]
    ROT = [0, 1, 2, 4, 5]

    def nextbank():
        b = ROT[rot[0] % 5]
        rot[0] += 1
        return b

    def bk(b):
        return ("ps", b)

    def dma(eng, out, in_, reads, writes):
        S.add(eng, lambda e, o=out, i=in_: e.dma_start(out=o, in_=i), reads, writes, dma=True)

    def act(out, in_, func, reads, writes, bias=None, scale=None):
        kw = {}
        if bias is not None:
            kw["bias"] = bias
        if scale is not None:
            kw["scale"] = scale
        S.add("act", lambda e, o=out, i=in_, f=func, kw=kw: e.activation(out=o, in_=i, func=f, **kw),
              reads, writes)

    def tt(eng, out, in0, in1, op, reads, writes):
        S.add(eng, lambda e, o=out, a=in0, b=in1, p=op: e.tensor_tensor(out=o, in0=a, in1=b, op=p),
              reads, writes)

    def ts(eng, out, in0, s1, op0, reads, writes, s2=None, op1=None):
        if op1 is None:
            S.add(eng, lambda e, o=out, a=in0, s=s1, p=op0: e.tensor_scalar(out=o, in0=a, scalar1=s, scalar2=None, op0=p),
                  reads, writes)
        else:
            S.add(eng, lambda e, o=out, a=in0, s=s1, p=op0, s_2=s2, p1=op1:
                  e.tensor_scalar(out=o, in0=a, scalar1=s, scalar2=s_2, op0=p, op1=p1), reads, writes)

    def stt(out, in0, scalar, in1, op0, op1, reads, writes):
        S.add("dve", lambda e, o=out, a=in0, s=scalar, b=in1, p0=op0, p1=op1:
              e.scalar_tensor_tensor(out=o, in0=a, scalar=s, in1=b, op0=p0, op1=p1), reads, writes)

    def copy(eng, out, in_, reads, writes):
        if eng == "act":
            S.add("act", lambda e, o=out, i=in_: e.copy(out=o, in_=i), reads, writes)
        else:
            S.add(eng, lambda e, o=out, i=in_: e.tensor_copy(out=o, in_=i), reads, writes)

    def mm_group(out, okey, pairs, reads):
        n = len(pairs)
        for i, pr in enumerate(pairs):
            l, r = pr[0], pr[1]
            rk = list(reads) + (list(pr[2]) if len(pr) > 2 else [])
            S.add("pe", lambda e, o=out, l=l, r=r, st=(i == 0), sp=(i == n - 1):
                  e.matmul(o, lhsT=l, rhs=r, start=st, stop=sp), rk, [okey])

    def mm1(out, okey, l, r, reads, start=True, stop=True):
        S.add("pe", lambda e, o=out, l=l, r=r, st=start, sp=stop: e.matmul(o, lhsT=l, rhs=r, start=st, stop=sp),
              reads, [okey])

    def transp(out, okey, in_, reads):
        S.add("pe", lambda e, o=out, i=in_: e.transpose(o, i, ident), list(reads) + ["ident"], [okey])

    def v3(ap, a):
        return ap.rearrange("p (a b) -> p a b", a=a)

    def blk_src(b):
        dst = scr[b]
        if b < 10:
            return [(v3(dst, 8), w_in.rearrange("(c p) n -> p c n", p=128)[:, :, b * 512:(b + 1) * 512])]
        if b < 12:
            k = b - 10
            d4 = dst.rearrange("p (s c n) -> p s c n", s=2, c=4)
            return [(d4[:, 0], w_a.rearrange("(c p) n -> p c n", p=128)[:, :, k * 512:(k + 1) * 512]),
                    (d4[:, 1], w_b.rearrange("(c p) n -> p c n", p=128)[:, :, k * 512:(k + 1) * 512])]
        if b < 14:
            k = b - 12
            return [(v3(dst, 8), w_out.rearrange("(c p) n -> p c n", p=128)[:, :, k * 512:(k + 1) * 512])]
        if b < 22:
            k = b - 14
            return [(v3(dst, 8), w1.rearrange("(c p) n -> p c n", p=128)[:, :, k * 512:(k + 1) * 512])]
        j = b - 22
        d3 = v3(dst, 32)
        s3 = w2.rearrange("(f p) n -> p f n", p=128)[:, :, j * 128:(j + 1) * 128]
        return [(d3[:, q * 8:(q + 1) * 8, :], s3[:, q * 8:(q + 1) * 8, :]) for q in range(4)]

    dbg = dbg or set()
    cast_rest = [1, 2, 5, 10, 6, 8, 11, 7, 9] + list(range(12, 30))

    def cast_block(b):
        if b >= 22:
            q = b - 22
            dma("pool", scrw2[:, q * 4:(q + 1) * 4, :],
                w2.rearrange("(f p) n -> p f n", p=128)[:, q * 4:(q + 1) * 4, :], [], [("scrw2", q)])
            return
        for (dv, sv) in blk_src(b):
            dma("pool", dv, sv, [], [("scr", b)])

    def cast_quantum():
        if cast_rest:
            cast_block(cast_rest.pop(0))

    for b in (0, 3, 4):
        cast_block(b)

    ring_free = [0, 1, 2, 3]

    def wload(b):
        s = ring_free.pop(0)
        if b >= 22:
            j = b - 22
            dma("sp", wring[:, s, :].rearrange("p (f n) -> p f n", f=32), scrw2[:, :, j * 128:(j + 1) * 128],
                [("scrw2", q) for q in range(8)], [("w", s)])
        else:
            dma("sp", wring[:, s, :], scr[b], [("scr", b)], [("w", s)])
        return wring[:, s, :], ("w", s), s

    def wfree(s):
        ring_free.append(s)

    dma("sp", xT[0], xT_d.rearrange("(c p) t -> p c t", p=128)[:, :, 0:T], [], [("xT", 0, c) for c in range(8)])
    dma("sp", cos_s[0], cos_d[:, 0:4, :], [], [("cos", 0)])
    dma("sp", sin_s[0], sin_d[:, 0:4, :], [], [("sin", 0)])
    dma("sp", pk1, pk1_d, [], ["g1", "g2", "gf", "convw", "convb", "br", "bi", "lam", "flag", "kdec", "kdecN"])
    dma("sp", wr32, wr_d, [], ["rt1"])
    dma("sp", wi32, wi_d, [], ["rt2"])
    dma("sp", pk2, pk2_d, [], ["dtab", "qdec", "cdt", "maskbd"])
    dma("pool", ident, ident_d, [], ["ident"])
    S.add("dve", lambda e: e.memset(ones, 1.0 / 1024.0), [], ["ones"])
    S.add("dve", lambda e: e.memset(epsc, EPS), [], ["epsc"])
    S.add("dve", lambda e: e.memset(onec, 1.0), [], ["onec"])
    S.add("dve", lambda e: e.memset(hist, 0.0), [], ["hist"])
    S.add("dve", lambda e: e.memset(hstate, 0.0), [], ["hstate"])
    S.add("dve", lambda e: e.memset(st32, 0.0), [], ["st32"])
    S.add("dve", lambda e: e.memset(stbf[0], 0.0), [], [("stbf", 0)])
    copy("dve", wrbd, wr32, ["rt1"], ["wrbd"])
    copy("dve", wibd, wi32, ["rt2"], ["wibd"])
    act(n8sp, lam, AF.Exp, ["lam"], ["n8sp"], scale=-1.0)
    act(n8sp, n8sp, AF.Ln, ["n8sp", "onec"], ["n8sp"], bias=onec[:, 0:1], scale=1.0)
    ts("dve", n16sp, n8sp, -16.0, ALU.mult, ["n8sp"], ["n16sp"])
    ts("dve", n4sp, n8sp, -4.0, ALU.mult, ["n8sp"], ["n4sp"])
    ts("dve", n8sp, n8sp, -8.0, ALU.mult, ["n8sp", "n16sp", "n4sp"], ["n8sp"])
    ts("dve", hbr, br, 0.5, ALU.mult, ["br"], ["hbr"])
    ts("dve", hbi, bi, 0.5, ALU.mult, ["bi"], ["hbi"])
    S.add("dve", lambda e: e.memset(qtrc, 0.25), [], ["qtrc"])

    def xkeys(xb):
        return [("xT", xb, c) for c in range(8)]

    def hkeys():
        return [("hT", c) for c in range(8)]

    def load_x(st_idx):
        xb = st_idx % 2
        src = xT_d.rearrange("(c p) t -> p c t", p=128)[:, :, st_idx * T:(st_idx + 1) * T]
        dma("sp", xT[xb], src, [], xkeys(xb))
        cb = st_idx % 2
        dma("sp", cos_s[cb], cos_d[:, st_idx * 4:(st_idx + 1) * 4, :], [], [("cos", cb)])
        dma("sp", sin_s[cb], sin_d[:, st_idx * 4:(st_idx + 1) * 4, :], [], [("sin", cb)])

    def norm_stats_chunk(xb, c, sbank):
        act(sqb[:, c, :], xT[xb][:, c, :], AF.Square, [("xT", xb, c)], [("sq", c)])
        S.add("pe", lambda e, c=c, sbank=sbank: e.matmul(banks[sbank], lhsT=ones, rhs=sqb[:, c, :],
                                                         start=(c == 0), stop=(c == 7)),
              ["ones", ("sq", c)], [bk(sbank)])

    def norm_finish(sbank, w):
        act(rstd2[w], banks[sbank], AF.Ln, [bk(sbank), "epsc"], [("rstd", w)], bias=epsc[:, 0:1], scale=1.0)
        act(rstd2[w], rstd2[w], AF.Exp, [("rstd", w)], [("rstd", w)], scale=-0.5)

    def norm_apply(xb, gcol, gkey, w, to_x, after=None):
        for c in range(8):
            xk = ("xT", xb, c)
            if to_x:
                stt(xT[xb][:, c, :], xT[xb][:, c, :], gcol[:, c:c + 1], rstd2[w], ALU.mult, ALU.mult,
                    [xk, gkey, ("rstd", w)], [xk])
                if after is not None:
                    after(c)
            else:
                stt(hT[:, c, :], xT[xb][:, c, :], gcol[:, c:c + 1], rstd2[w], ALU.mult, ALU.mult,
                    [xk, gkey, ("rstd", w)], [("hT", c)])

    def norm1_stats(st_idx):
        xb = st_idx % 2
        for c in range(8):
            norm_stats_chunk(xb, c, 6)
        norm_finish(6, 1)

    tokbank = [0]

    def tok_unit(nm, t, wb_, kw, main, cb, st_idx, bpair=(5, 7)):
        pb = bpair[tokbank[0] % len(bpair)]
        tokbank[0] += 1
        mm_group(banks[pb], bk(pb), [(hT[:, c, t * 128:(t + 1) * 128], wb_[:, c, :], [("hT", c)]) for c in range(8)],
                 [kw])
        P3 = banks[pb].rearrange("p (h d) -> p h d", h=8)
        if nm in ("q", "k"):
            dst = q_r[t] if nm == "q" else k_r[t]
            dkey = ("q_r", t) if nm == "q" else ("k_r", t)
            cs = cos_s[cb][:, t, :].unsqueeze(1).broadcast_to([128, 8, 64])
            s_lo = sin_s[cb][:, t, 0:32].unsqueeze(1).broadcast_to([128, 8, 32])
            s_hi = sin_s[cb][:, t, 32:64].unsqueeze(1).broadcast_to([128, 8, 32])
            r1 = rt1.rearrange("p (h d) -> p h d", h=8)
            r2 = rt2.rearrange("p (h d) -> p h d", h=8)
            tt("dve", r1, P3, cs, ALU.mult, [bk(pb), ("cos", cb)], ["rt1"])
            tt("dve", r2[:, :, 0:32], P3[:, :, 32:64], s_lo, ALU.mult, [bk(pb), ("sin", cb)], ["rt2"])
            tt("dve", r2[:, :, 32:64], P3[:, :, 0:32], s_hi, ALU.mult, [bk(pb), ("sin", cb)], ["rt2"])
            tt("pool", dst, rt1, rt2, ALU.add, ["rt1", "rt2"], [dkey])
            if nm == "k":
                if main:
                    kd3 = kdec.unsqueeze(2).broadcast_to([128, 8, 64])
                else:
                    kd3 = kdecN[:, st_idx * 4 + t, :].unsqueeze(2).broadcast_to([128, 8, 64])
                tt("pool", kdc[t].rearrange("p (h d) -> p h d", h=8),
                   k_r[t].rearrange("p (h d) -> p h d", h=8), kd3, ALU.mult,
                   [("k_r", t), "kdec", "kdecN"], [("kdc", t)])
        elif nm == "v":
            copy("act", v_t[t], banks[pb], [bk(pb)], [("v_t", t)])
        else:
            act(sgr[t], banks[pb], AF.Silu, [bk(pb)], [("sgr", t)])

    def lru_pieces(main, st_idx):
        W = {}

        def load():
            w_, k_, sl = wload(0)
            W["xa"] = (v3(w_, 8), k_, sl)
            if main:
                w_, k_, sl = wload(1)
                W["ga"] = (v3(w_, 8), k_, sl)

        def A(j):
            jb = j % 2
            wxa, kxa, _ = W["xa"]
            mm_group(banks[0], bk(0), [(wxa[:, c, j * 128:(j + 1) * 128], hT[:, c, :], [("hT", c)]) for c in range(8)],
                     [kxa])
            copy("act", xa_ext[jb][:, 3:T + 3], banks[0], [bk(0)], [("xa_ext", jb)])
            copy("pool", xa_ext[jb][:, 0:3], hist[:, j, :], ["hist"], [("xa_ext", jb)])
            ts("dve", xc[jb], xa_ext[jb][:, 0:T], convw[:, j, 0:1], ALU.mult,
               [("xa_ext", jb), "convw", "convb"], [("xc", jb)], s2=convb[:, j:j + 1], op1=ALU.add)
            for k in range(1, 4):
                stt(xc[jb], xa_ext[jb][:, k:k + T], convw[:, j, k:k + 1], xc[jb], ALU.mult, ALU.add,
                    [("xa_ext", jb), "convw", ("xc", jb)], [("xc", jb)])
            copy("pool", hist[:, j, :], xa_ext[jb][:, T:T + 3], [("xa_ext", jb)], ["hist"])
            copy("dve", xcb[jb], xc[jb], [("xc", jb)], [("xcb", jb)])
            if main:
                wga, kga, _ = W["ga"]
                mm_group(banks[4], bk(4), [(wga[:, c, j * 128:(j + 1) * 128], hT[:, c, :], [("hT", c)]) for c in range(8)],
                         [kga])
                act(gg[jb], banks[4], AF.Gelu_apprx_tanh, [bk(4)], [("gg", jb)])
            if j == 3:
                wfree(W["xa"][2])
                if main:
                    wfree(W["ga"][2])

        def B(j):
            jb = j % 2
            mm1(banks[2], bk(2), wrbd[:, j, :], xcb[jb], ["wrbd", ("xcb", jb)])
            mm1(banks[6], bk(6), wibd[:, j, :], xcb[jb], ["wibd", ("xcb", jb)])
            act(L2[jb], banks[2], AF.Tanh, [bk(2), "hbr"], [("L2", jb)], bias=hbr[:, j:j + 1], scale=0.5)
            act(L3[jb], banks[6], AF.Tanh, [bk(6), "hbi"], [("L3", jb)], bias=hbi[:, j:j + 1], scale=0.5)
            act(L4[jb], L2[jb], AF.Exp, [("L2", jb), "n4sp"], [("L4", jb)], bias=n4sp[:, j:j + 1],
                scale=n4sp[:, j:j + 1])
            act(L2[jb], L2[jb], AF.Exp, [("L2", jb), "n8sp"], [("L2", jb)], bias=n8sp[:, j:j + 1],
                scale=n8sp[:, j:j + 1])
            act(L2[jb], L2[jb], AF.Sqrt, [("L2", jb), "qtrc"], [("L2", jb)], bias=qtrc[:, 0:1], scale=-0.25)
            stt(L3[jb], L3[jb], 1.0, xc[jb], ALU.add, ALU.mult, [("L3", jb), ("xc", jb)], [("L3", jb)])
            tt("pool", L3[jb], L3[jb], L2[jb], ALU.mult, [("L3", jb), ("L2", jb)], [("L3", jb)])
            S.add("dve", lambda e, jb=jb, j=j: e.tensor_tensor_scan(
                out=L5[jb], data0=L4[jb], data1=L3[jb], initial=hstate[:, j:j + 1], op0=ALU.mult, op1=ALU.add),
                [("L4", jb), ("L3", jb), "hstate"], [("L5", jb)])
            copy("pool", hstate[:, j:j + 1], L5[jb][:, T - 1:T], [("L5", jb)], ["hstate"])
            if main:
                tt("pool", yaT[:, j, :], gg[jb], L5[jb], ALU.mult, [("gg", jb), ("L5", jb)], ["yaT"])
                if st_idx == NST:
                    cast_quantum()

        return load, A, B

    def tok_blocks(names):
        bid = {"q": 2, "k": 3, "v": 4, "gr": 5}
        out = []
        for nm in names:
            w_, k_, sl = wload(bid[nm])
            out.append((nm, v3(w_, 8), k_, sl))
        return out

    def lru_standalone(main, st_idx, cb):
        load, A, B = lru_pieces(main, st_idx)
        load()
        fw = tok_blocks(("q", "v") if main else ("k", "v"))
        for j in range(5):
            if j < 4:
                A(j)
                for (nm, w_, k_, sl) in fw:
                    tok_unit(nm, j, w_, k_, main, cb, st_idx, bpair=(5, 7) if main else (5, 7, 4))
            if j >= 1:
                B(j - 1)
        for (nm, w_, k_, sl) in fw:
            wfree(sl)
        if main:
            rest = tok_blocks(("k", "gr"))
            for t in range(4):
                for (nm, w_, k_, sl) in rest:
                    tok_unit(nm, t, w_, k_, main, cb, st_idx)
            for (nm, w_, k_, sl) in rest:
                wfree(sl)
        else:
            cast_quantum()
            cast_quantum()
            cast_quantum()

    PEND = []

    def tok_phase(st_idx, cb):
        blks = tok_blocks(("q", "v", "k", "gr"))
        for t in range(4):
            for (nm, w_, k_, sl) in blks:
                tok_unit(nm, t, w_, k_, True, cb, st_idx, bpair=(0, 1))
            if t == 0 and PEND:
                PEND.pop(0)()
            if t >= 1:
                ret_A(t - 1, (t - 1) % 2)
            if t >= 2:
                ret_B(t - 2, (t - 2) % 2)
        for (nm, w_, k_, sl) in blks:
            wfree(sl)
        ret_A(3, 1)
        ret_B(2, 0)
        ret_B(3, 1)

    stv = [0]

    def kv_update(t):
        cur = stv[0] % 2
        nxt = 1 - cur
        kvp = banks[7].rearrange("p (j n) -> p j n", j=4)
        tt("pool", st32, st32, cdt, ALU.mult, ["st32", "cdt"], ["st32"])
        for j in range(4):
            mm1(kvp[:, j, :], bk(7), kdc[t][:, j * 128:(j + 1) * 128], v_t[t][:, j * 128:(j + 1) * 128],
                [("kdc", t), ("v_t", t)])
        tt("dve", kvt, kvp, maskbd, ALU.mult, [bk(7), "maskbd"], ["kvt"])
        tt("dve", st32, st32, kvt, ALU.add, ["st32", "kvt"], ["st32"])
        copy("act", stbf[nxt], st32, ["st32"], [("stbf", nxt)])
        stv[0] += 1

    def kv_prepass():
        kvp = banks[7].rearrange("p (j n) -> p j n", j=4)
        for j in range(4):
            for t in range(4):
                mm1(kvp[:, j, :], bk(7), kdc[t][:, j * 128:(j + 1) * 128], v_t[t][:, j * 128:(j + 1) * 128],
                    [("kdc", t), ("v_t", t)], start=(t == 0), stop=(t == 3))
        tt("dve", kvt, kvp, maskbd, ALU.mult, [bk(7), "maskbd"], ["kvt"])
        tt("dve", st32, st32, kvt, ALU.add, ["st32", "kvt"], ["st32"])

    def ret_A(t, sci):
        pT = pT_all
        for j in range(4):
            transp(pT[:, j, :], bk(3), q_r[t][:, j * 128:(j + 1) * 128], [("q_r", t)])
        for j in range(4):
            transp(pT[:, 4 + j, :], bk(3), k_r[t][:, j * 128:(j + 1) * 128], [("k_r", t)])
        copy("act", qTs, pT[:, 0:4, :], [bk(3)], ["qTs"])
        copy("act", kTs, pT[:, 4:8, :], [bk(3)], ["kTs"])
        tt("dve", qTd[sci], pT[:, 0:4, :], qdec, ALU.mult, [bk(3), "qdec"], [("qTd", sci)])
        for h in range(8):
            par, j = h % 2, h // 2
            o = par * 64
            mm1(banks[4 + par][:, j * 128:(j + 1) * 128], bk(4 + par), kTs[o:o + 64, j, :], qTs[o:o + 64, j, :],
                ["kTs", "qTs"])
        sc = scT[sci]
        for par in range(2):
            tt("dve", sc[:, par].rearrange("p j n -> p (j n)"), banks[4 + par],
               dtab[:, par].rearrange("p j n -> p (j n)"), ALU.mult, [bk(4 + par), "dtab"], [("scT", sci)])

    def ret_B(t, sci):
        cur = stv[0] % 2
        ob = (6, 2)[t % 2]
        pT = pT_all
        sc = scT[sci]
        for j in range(4):
            mm1(banks[ob][:, j * 128:(j + 1) * 128], bk(ob), qTd[sci][:, j, :], stbf[cur][:, j, :],
                [("qTd", sci), ("stbf", cur)], start=True, stop=False)
            for par in range(2):
                h = 2 * j + par
                mm1(banks[ob][:, h * 64:(h + 1) * 64], bk(ob), sc[:, par, j, :], v_t[t][:, h * 64:(h + 1) * 64],
                    [("scT", sci), ("v_t", t)], start=False, stop=(par == 1))
        kv_update(t)
        act(sqr, banks[ob], AF.Square, [bk(ob)], ["sqr"])
        S.add("dve", lambda e: e.tensor_reduce(out=ssq, in_=sqr.rearrange("p (h d) -> p h d", h=8),
                                               op=ALU.add, axis=AX.X), ["sqr"], ["ssq"])
        act(ssq, ssq, AF.Ln, ["ssq", "epsc"], ["ssq"], bias=epsc[:, 0:1], scale=1.0 / 64.0)
        act(ssq, ssq, AF.Exp, ["ssq"], ["ssq"], scale=-0.5)
        tt("dve", tmp2.rearrange("p (h d) -> p h d", h=8), banks[ob].rearrange("p (h d) -> p h d", h=8),
           ssq.unsqueeze(2).broadcast_to([128, 8, 64]), ALU.mult, [bk(ob), "ssq"], ["tmp2"])
        tt("pool", ybt, tmp2, sgr[t], ALU.mult, ["tmp2", ("sgr", t)], ["ybt"])
        for j in range(4):
            transp(pT[:, j, :], bk(3), ybt[:, j * 128:(j + 1) * 128], ["ybt"])
        copy("act", ybT[:, :, t * 128:(t + 1) * 128], pT[:, 0:4, :], [bk(3)], ["ybT"])

    def retention():
        for f, t, b in ((ret_A, 0, 0), (ret_A, 1, 1), (ret_B, 0, 0), (ret_A, 2, 0), (ret_B, 1, 1), (ret_A, 3, 1),
                        (ret_B, 2, 0), (ret_B, 3, 1)):
            f(t, b)
            cast_quantum()

    def merge(xb):
        for half in range(2):
            wab, kab, sl_ab = wload(10 + half)
            wab = wab.rearrange("p (s c n) -> p s c n", s=2, c=4)
            wsa, ksa, sl_sa = wload(6 + half)
            wsa = v3(wsa, 8)
            wsb, ksb, sl_sb = wload(8 + half)
            wsb = v3(wsb, 8)
            for jj in range(4):
                j = half * 4 + jj
                jb = j % 2
                cs = slice(jj * 128, (jj + 1) * 128)
                bsa, bsb, bpa, bpb = nextbank(), nextbank(), nextbank(), nextbank()
                mm_group(banks[bsa], bk(bsa), [(wsa[:, c, cs], hT[:, c, :], [("hT", c)]) for c in range(8)], [ksa])
                mm_group(banks[bsb], bk(bsb), [(wsb[:, c, cs], hT[:, c, :], [("hT", c)]) for c in range(8)], [ksb])
                mm_group(banks[bpa], bk(bpa), [(wab[:, 0, c, cs], yaT[:, c, :]) for c in range(4)], [kab, "yaT"])
                mm_group(banks[bpb], bk(bpb), [(wab[:, 1, c, cs], ybT[:, c, :]) for c in range(4)], [kab, "ybT"])
                act(siga[jb], banks[bsa], AF.Sigmoid, [bk(bsa)], [("siga", jb)])
                act(sigb[jb], banks[bsb], AF.Sigmoid, [bk(bsb)], [("sigb", jb)])
                tt("dve", siga[jb], banks[bpa], siga[jb], ALU.mult, [bk(bpa), ("siga", jb)], [("siga", jb)])
                tt("dve", sigb[jb], banks[bpb], sigb[jb], ALU.mult, [bk(bpb), ("sigb", jb)], [("sigb", jb)])
                tt("pool", mT[:, j, :], siga[jb], sigb[jb], ALU.add, [("siga", jb), ("sigb", jb)], [("mT", j)])
            wfree(sl_ab)
            wfree(sl_sa)
            wfree(sl_sb)
            cast_quantum()
            cast_quantum()
        for half in range(2):
            wo, ko, sl_o = wload(12 + half)
            wo = v3(wo, 8)
            for jj in range(4):
                j = half * 4 + jj
                pb = nextbank()
                mm_group(banks[pb], bk(pb), [(wo[:, c, jj * 128:(jj + 1) * 128], mT[:, c, :], [("mT", c)]) for c in range(8)],
                         [ko])
                tt("dve", xT[xb][:, j, :], banks[pb], xT[xb][:, j, :], ALU.add, [bk(pb), ("xT", xb, j)],
                   [("xT", xb, j)])
                if j >= 1:
                    norm_stats_chunk(xb, j - 1, 7)
            wfree(sl_o)
        norm_stats_chunk(xb, 7, 7)
        norm_finish(7, 0)

    LRU_SCHED = [[("A", 0)], [("A", 1)], [("B", 0)], [("A", 2)], [("B", 1)], [("A", 3)], [("B", 2)], [("B", 3)]]

    def ffn(xb, st_idx):
        n = 0
        for b in range(8):
            wv, kw, sl = wload(14 + b)
            wv = v3(wv, 8)
            for i in range(4):
                pb = nextbank()
                rb = n % 2
                n += 1
                mm_group(banks[pb], bk(pb), [(wv[:, c, i * 128:(i + 1) * 128], hT[:, c, :], [("hT", c)]) for c in range(8)],
                         [kw])
                act(rl[rb], banks[pb], AF.Relu, [bk(pb)], [("rl", rb)])
                tt("pool", hidT[:, b * 4 + i, :], rl[rb], rl[rb], ALU.mult, [("rl", rb)], [("hid", b * 4 + i)])
            wfree(sl)
            if b == 4 and st_idx + 1 < n_st:
                norm1_stats(st_idx + 1)
        overlap = st_idx + 1 < n_st
        if overlap:
            norm_apply((st_idx + 1) % 2, g1, "g1", 1, to_x=False)
            load, A, B = lru_pieces(True, st_idx + 1)
        for j in range(8):
            wv, kw, sl = wload(22 + j)
            wv = v3(wv, 32)
            pb = (1, 5)[j % 2]
            mm_group(banks[pb], bk(pb), [(wv[:, f, :], hidT[:, f, :], [("hid", f)]) for f in range(32)], [kw])
            wfree(sl)
            tt("dve", xT[xb][:, j, :], banks[pb], xT[xb][:, j, :], ALU.add, [bk(pb), ("xT", xb, j)],
               [("xT", xb, j)])
            if j >= 1:
                norm_stats_chunk(xb, j - 1, 7)
            if overlap:
                if j == 0:
                    load()
                for kind, jj in LRU_SCHED[j]:
                    (A if kind == "A" else B)(jj)
        norm_stats_chunk(xb, 7, 7)
        norm_finish(7, 0)

    sci = [0]
    norm1_stats(0)
    for st_idx in range(n_st):
        main = st_idx >= NST
        xb = st_idx % 2
        cb = st_idx % 2
        if st_idx == NST:
            while cast_rest and cast_rest[0] < 14:
                cast_quantum()
            ts("dve", hstate, hstate, flag[:, 0:1], ALU.mult, ["hstate", "flag"], ["hstate"])
        if st_idx == 0:
            norm_apply(xb, g1, "g1", 1, to_x=False)
        if st_idx <= NST:
            lru_standalone(main, st_idx, cb)
        else:
            tok_phase(st_idx, cb)
        if st_idx + 1 < n_st:
            load_x(st_idx + 1)
        if main:
            if st_idx == NST:
                retention()
            merge(xb)
            norm_apply(xb, g2, "g2", 0, to_x=False)
            while cast_rest:
                cast_quantum()
            ffn(xb, st_idx)
            m = st_idx - NST
            dst = outT_d.rearrange("(c p) t -> p c t", p=128)[:, :, m * T:(m + 1) * T]
            if st_idx == n_st - 1:
                norm_apply(xb, gf, "gf", 0, to_x=True,
                           after=lambda c: dma("act", dst[:, c, :], xT[xb][:, c, :], [("xT", xb, c)], [("out", m, c)]))
            else:
                norm_apply(xb, gf, "gf", 0, to_x=True)
                PEND.append(lambda dst=dst, xb=xb, m=m: dma("act", dst, xT[xb], xkeys(xb), [("out", m)]))
        else:
            if st_idx + 1 < n_st:
                norm1_stats(st_idx + 1)
                norm_apply((st_idx + 1) % 2, g1, "g1", 1, to_x=False)
            kv_prepass()
            if st_idx == NST - 1:
                copy("act", stbf[stv[0] % 2], st32, ["st32"], [("stbf", stv[0] % 2)])
        S.new_epoch()
    S.emit()
    return nc


def _const_tables():
    nh, dh, C = 8, 64, 128
    log_g = np.log1p(-np.exp2(-5.0 - np.arange(nh, dtype=np.float64)))
    pos = np.arange(C, dtype=np.float64)
    rel = pos[None, :] - pos[:, None]
    dtab = np.zeros((C, 2, 4, C), np.float64)
    for h in range(nh):
        dtab[:, h % 2, h // 2, :] = np.where(rel >= 0, np.exp(log_g[h] * np.maximum(rel, 0.0)), 0.0) * 0.125
    qdec = np.zeros((128, 4, C), np.float64)
    cdt = np.zeros((128, 4, 128), np.float64)
    mask = np.zeros((128, 4, 128), np.float64)
    for p in range(128):
        for j in range(4):
            h = 2 * j + p // 64
            qdec[p, j, :] = np.exp(log_g[h] * (pos + 1.0))
            cdt[p, j, :] = np.exp(log_g[h] * C)
            mask[p, j, (p // 64) * 64:(p // 64) * 64 + 64] = 1.0
    kdec = np.zeros((C, nh), np.float64)
    for h in range(nh):
        kdec[:, h] = np.exp(log_g[h] * (C - 1.0 - pos)) * 0.125
    kdecN = np.zeros((C, 16, nh), np.float64)
    for n in range(16):
        for h in range(nh):
            kdecN[:, n, h] = np.exp(log_g[h] * (C - 1.0 - pos + C * (15 - n))) * 0.125
    ident = np.eye(128, dtype=np.float32)
    return (dtab.astype(np.float32), qdec.astype(np.float32), kdec.astype(np.float32),
            cdt.astype(np.float32), mask.astype(np.float32), ident, kdecN.astype(np.float32))


def _rope_tables(pos0):
    half = 32
    inv = 10000.0 ** (-np.arange(half, dtype=np.float64) / half)
    p = np.arange(128)[:, None] + np.arange(32)[None, :] * 128 + pos0
    ang = p.astype(np.float64)[:, :, None] * inv[None, None, :]
    c, s = np.cos(ang), np.sin(ang)
    cos2 = np.concatenate([c, c], axis=-1).astype(np.float32)
    sin2 = np.concatenate([-s, s], axis=-1).astype(np.float32)
    return np.ascontiguousarray(cos2), np.ascontiguousarray(sin2)


def kernel(x, norm1_g, w_in, conv_w, conv_b, lru_wr, lru_br, lru_wi, lru_bi, lru_lambda,
           w_branch_a, w_branch_b, w_out, norm2_g, w_ff1, w_ff2, norm_f_g):
    f32 = np.float32
    x = np.asarray(x, f32)

    def pc(v, n):
        return np.ascontiguousarray(np.asarray(v, f32).reshape(n, 128).T)

    dtab, qdec, kdec, cdt, mask, ident, kdecN = _const_tables()
    shared = {
        "w_in": np.ascontiguousarray(np.asarray(w_in, f32)[0]),
        "w_a": np.ascontiguousarray(np.asarray(w_branch_a, f32)[0]),
        "w_b": np.ascontiguousarray(np.asarray(w_branch_b, f32)[0]),
        "w_out": np.ascontiguousarray(np.asarray(w_out, f32)[0]),
        "w1": np.ascontiguousarray(np.asarray(w_ff1, f32)[0]),
        "w2": np.ascontiguousarray(np.asarray(w_ff2, f32)[0]),
    }
    wr_bd = np.zeros((128, 4, 128), f32)
    wi_bd = np.zeros((128, 4, 128), f32)
    wr0, wi0 = np.asarray(lru_wr, f32)[0], np.asarray(lru_wi, f32)[0]
    for blk in range(8):
        j, o = blk // 2, (blk % 2) * 64
        wr_bd[o:o + 64, j, o:o + 64] = wr0[blk]
        wi_bd[o:o + 64, j, o:o + 64] = wi0[blk]
    shared["wr"] = wr_bd
    shared["wi"] = wi_bd
    shared["ident"] = ident
    pk2 = np.concatenate([dtab.reshape(128, -1), qdec.reshape(128, -1), cdt.reshape(128, -1),
                          mask.reshape(128, -1)], axis=1).astype(f32)
    shared["pk2"] = np.ascontiguousarray(pk2)

    def pack1(flagval):
        pk = np.zeros((128, PK1), f32)
        pk[:, 0:8] = pc(np.asarray(norm1_g)[0], 8)
        pk[:, 8:16] = pc(np.asarray(norm2_g)[0], 8)
        pk[:, 16:24] = pc(norm_f_g, 8)
        pk[:, 24:40] = np.asarray(conv_w, f32)[0].reshape(4, 4, 128).transpose(2, 1, 0).reshape(128, 16)
        pk[:, 40:44] = pc(np.asarray(conv_b)[0], 4)
        pk[:, 44:48] = pc(np.asarray(lru_br)[0].reshape(-1), 4)
        pk[:, 48:52] = pc(np.asarray(lru_bi)[0].reshape(-1), 4)
        pk[:, 52:56] = pc(np.asarray(lru_lambda)[0], 4)
        pk[:, 56] = flagval
        pk[:, 64:72] = kdec
        pk[:, 72:200] = kdecN.reshape(128, 128)
        return pk

    in_maps = []
    for c in range(8):
        b, half = c // 2, c % 2
        xt = np.zeros((D, 2 * TOK), f32)
        if half == 1:
            xt[:, :TOK] = x[b, :TOK].T
        xt[:, TOK:] = x[b, half * TOK:(half + 1) * TOK].T
        cos2, sin2 = _rope_tables(0 if half == 1 else -TOK)
        m = dict(shared)
        m["xT"] = xt
        m["pk1"] = pack1(float(half))
        m["cos2"] = cos2
        m["sin2"] = sin2
        in_maps.append(m)
    nc = build_nc()
    res = run_bass_kernel_spmd(nc, in_maps, core_ids=list(range(8)))
    out = np.empty((NB, SEQ, D), f32)
    for c in range(8):
        b, half = c // 2, c % 2
        out[b, half * TOK:(half + 1) * TOK, :] = np.asarray(res.results[c]["outT"], f32).T
    return out
```

```python
import contextlib
import numpy as np
import concourse.bass as bass
import concourse.mybir as mybir
from concourse.bass_utils import run_bass_kernel_spmd

F32 = mybir.dt.float32
BF16 = mybir.dt.bfloat16
AF = mybir.ActivationFunctionType
ALU = mybir.AluOpType
AX = mybir.AxisListType

D = 1024
SEQ = 4096
NB = 4
TOK = 2048
T = 512
NST = TOK // T
EPS = 1e-6
NBLK = 30
PK1 = 200
PK2 = 2560


class _Op:
    __slots__ = ("eng", "fn", "deps", "idx", "is_dma", "sem", "semval", "milestone",
                 "mnum", "epoch", "prewait")


class Sched:
    ENGS = ("pe", "act", "dve", "pool", "sp")
    DMA_KS = {"pe": 8, "act": 8, "dve": 8, "pool": 48, "sp": 8}

    def __init__(self, nc):
        self.nc = nc
        self.ops = {e: [] for e in self.ENGS}
        self.lastw = {}
        self.readers = {}
        self.epoch = 0
        self.dma_n = {e: 0 for e in self.ENGS}
        self.alias = {}
        self.bank_last = {}

    def add_alias(self, a, b):
        self.alias.setdefault(a, []).append(b)
        self.alias.setdefault(b, []).append(a)

    def new_epoch(self):
        self.epoch += 1

    def add(self, eng, fn, reads=(), writes=(), dma=False):
        op = _Op()
        op.eng = eng
        op.fn = fn
        op.is_dma = dma
        op.milestone = False
        op.mnum = 0
        op.epoch = self.epoch
        op.sem = None
        op.semval = 0
        op.prewait = None
        deps = {}
        for k in reads:
            w = self.lastw.get(k)
            if w is not None:
                deps[id(w)] = (w, True)
        for k in list(reads) + list(writes):
            if isinstance(k, tuple) and k and k[0] == "ps":
                bl = self.bank_last.setdefault(k, {})
                for e2, o2 in bl.items():
                    if e2 != eng and id(o2) not in deps:
                        deps[id(o2)] = (o2, False)
                bl[eng] = op
        if self.alias:
            wl = list(writes)
            for k in writes:
                for a in self.alias.get(k, ()):
                    if a not in wl:
                        wl.append(a)
            writes = wl
        for k in writes:
            w = self.lastw.get(k)
            if w is not None and id(w) not in deps:
                deps[id(w)] = (w, False)
            rd = self.readers.get(k)
            if rd:
                for r in rd[0].values():
                    if id(r) not in deps:
                        deps[id(r)] = (r, False)
                for r in rd[1]:
                    if id(r) not in deps:
                        deps[id(r)] = (r, False)
        for k in reads:
            rd = self.readers.get(k)
            if rd is None:
                rd = self.readers[k] = ({}, [])
            if dma:
                rd[1].append(op)
            else:
                rd[0][eng] = op
        for k in writes:
            self.lastw[k] = op
            self.readers[k] = ({}, [])
        fin = []
        for (d, raw) in deps.values():
            if d is op:
                continue
            if (not d.is_dma) and d.eng == eng and not dma:
                if eng == "pe":
                    continue
            fin.append(d)
        op.deps = fin
        lst = self.ops[eng]
        op.idx = len(lst)
        lst.append(op)
        if dma:
            i = self.dma_n[eng]
            self.dma_n[eng] = i + 1
            K = self.DMA_KS[eng]
            op.sem = (eng, i % K)
            op.semval = 16 * (i // K + 1)
            if i >= K:
                op.prewait = (op.sem, 16 * (i // K))
        return op

    def emit(self):
        nc = self.nc
        for e in self.ENGS:
            for op in self.ops[e]:
                for d in op.deps:
                    if not d.is_dma:
                        d.milestone = True
        nep = self.epoch + 1
        for e in self.ENGS:
            cnt = [0] * nep
            for op in self.ops[e]:
                if op.is_dma:
                    continue
                if op.milestone:
                    cnt[op.epoch] += 1
                    op.mnum = cnt[op.epoch]
        stack = contextlib.ExitStack()
        sems = {}
        with stack:
            for e in self.ENGS:
                for p in range(nep):
                    if any((not o.is_dma) and o.milestone and o.epoch == p for o in self.ops[e]):
                        sems[(e, p)] = stack.enter_context(nc.semaphore(f"s_{e}_{p}"))
                if self.dma_n[e]:
                    for k in range(min(self.DMA_KS[e], self.dma_n[e])):
                        sems[("dma", e, k)] = stack.enter_context(nc.semaphore(f"d_{e}_{k}"))
            block = stack.enter_context(nc.Block())
            hooks = {"pe": block.tensor, "act": block.scalar, "dve": block.vector,
                     "pool": block.gpsimd, "sp": block.sync}
            all_dma_last = {}
            for e in self.ENGS:
                for op in self.ops[e]:
                    if op.is_dma:
                        all_dma_last[("dma", e, op.sem[1])] = op.semval

            def mk(e):
                def body(eng):
                    waited = {}

                    def wait(key, val):
                        if waited.get(key, 0) >= val:
                            return
                        waited[key] = val
                        eng.wait_ge(sems[key], val)

                    for op in self.ops[e]:
                        for d in op.deps:
                            if d.is_dma:
                                wait(("dma", d.sem[0], d.sem[1]), d.semval)
                            else:
                                wait((d.eng, d.epoch), d.mnum)
                        if op.prewait is not None:
                            wait(("dma", op.prewait[0][0], op.prewait[0][1]), op.prewait[1])
                        ins = op.fn(eng)
                        if op.is_dma:
                            ins.then_inc(sems[("dma", op.sem[0], op.sem[1])], 16)
                        elif op.milestone:
                            ins.then_inc(sems[(e, op.epoch)], 1)
                    if e == "sp":
                        for key, val in all_dma_last.items():
                            wait(key, val)
                return body

            for e in self.ENGS:
                if self.ops[e] or e == "sp":
                    hooks[e](mk(e))


def build_nc(n_st=2 * NST, dbg=None):
    nc = bass.Bass("TRN2", target_bir_lowering=False)

    def din(name, shape, dt=F32):
        return nc.dram_tensor(name, list(shape), dt, kind="ExternalInput").ap()

    xT_d = din("xT", [D, 2 * TOK])
    w_in = din("w_in", [D, 5120])
    w_a = din("w_a", [512, D])
    w_b = din("w_b", [512, D])
    w_out = din("w_out", [D, D])
    w1 = din("w1", [D, 4096])
    w2 = din("w2", [4096, D])
    wr_d = din("wr", [128, 4, 128])
    wi_d = din("wi", [128, 4, 128])
    pk1_d = din("pk1", [128, PK1])
    pk2_d = din("pk2", [128, PK2])
    cos_d = din("cos2", [128, 32, 64])
    sin_d = din("sin2", [128, 32, 64])
    ident_d = din("ident", [128, 128])
    outT_d = nc.dram_tensor("outT", [D, TOK], F32, kind="ExternalOutput").ap()
    scr = nc.dram_tensor("scr", [22, 128, 4096], BF16, kind="Internal").ap()
    scrw2 = nc.dram_tensor("scrw2", [128, 32, 1024], BF16, kind="Internal").ap()

    S = Sched(nc)

    def sb(name, shape, dt=F32):
        return nc.alloc_sbuf_tensor("sb_" + name, list(shape), dt).ap()

    xT = [sb(f"xT{i}", [128, 8, T]) for i in range(2)]
    hT = sb("hT", [128, 8, T], BF16)
    rstd2 = [sb(f"rstd{i}", [128, T]) for i in range(2)]
    sqb = sb("sqb", [128, 8, T], BF16)
    wring = sb("wring", [128, 4, 4096], BF16)
    xa_ext = [sb(f"xa_ext{i}", [128, T + 3]) for i in range(2)]
    hist = sb("hist", [128, 4, 3])
    xc = [sb(f"xc{i}", [128, T]) for i in range(2)]
    xcb = [sb(f"xcb{i}", [128, T], BF16) for i in range(2)]
    L2 = [sb(f"L2_{i}", [128, T]) for i in range(2)]
    L3 = [sb(f"L3_{i}", [128, T]) for i in range(2)]
    L4 = [sb(f"L4_{i}", [128, T]) for i in range(2)]
    L5 = [sb(f"L5_{i}", [128, T]) for i in range(2)]
    gg = [sb(f"gg{i}", [128, T]) for i in range(2)]
    hstate = sb("hstate", [128, 4])
    yaT = sb("yaT", [128, 4, T], BF16)
    ybT = sb("ybT", [128, 4, T], BF16)
    hidT = sb("hidT", [128, 32, T], BF16)
    hflat = hidT.rearrange("p f t -> p (f t)")

    def ov(f0, nf, dt, key):
        v = hflat[:, f0 * T:(f0 + nf) * T]
        if dt == F32:
            v = v.bitcast(F32)
        for f in range(f0, f0 + nf):
            S.add_alias(key, ("hid", f))
        return v

    q_r = [ov(0 + t, 1, BF16, ("q_r", t)) for t in range(4)]
    k_r = [ov(4 + t, 1, BF16, ("k_r", t)) for t in range(4)]
    kdc = [ov(8 + t, 1, BF16, ("kdc", t)) for t in range(4)]
    v_t = [ov(12 + t, 1, BF16, ("v_t", t)) for t in range(4)]
    sgr = [ov(16 + 2 * t, 2, F32, ("sgr", t)) for t in range(4)]
    rt1 = ov(24, 2, F32, "rt1")
    rt2 = ov(26, 2, F32, "rt2")
    qTs = sb("qTs", [128, 4, 128], BF16)
    qTd = [sb(f"qTd{i}", [128, 4, 128], BF16) for i in range(2)]
    kTs = sb("kTs", [128, 4, 128], BF16)
    scT = [sb(f"scT{i}", [128, 2, 4, 128], BF16) for i in range(2)]
    sqr = ov(28, 2, F32, "sqr")
    tmp2 = ov(30, 2, F32, "tmp2")
    ybt = sb("ybt", [128, 512], BF16)
    ssq = sb("ssq", [128, 8])
    st32 = sb("st32", [128, 4, 128])
    stbf = [sb(f"stbf{i}", [128, 4, 128], BF16) for i in range(2)]
    kvt = sb("kvt", [128, 4, 128])
    siga = [sb(f"siga{i}", [128, T]) for i in range(2)]
    sigb = [sb(f"sigb{i}", [128, T]) for i in range(2)]
    mT = sb("mT", [128, 8, T], BF16)
    rl = [sb(f"rl{i}", [128, T]) for i in range(2)]
    cos_s = [sb(f"cos_s{i}", [128, 4, 64]) for i in range(2)]
    sin_s = [sb(f"sin_s{i}", [128, 4, 64]) for i in range(2)]
    pk1 = sb("pk1", [128, PK1])
    pk2 = sb("pk2", [128, PK2])
    g1, g2, gf = pk1[:, 0:8], pk1[:, 8:16], pk1[:, 16:24]
    convw = pk1[:, 24:40].rearrange("p (j k) -> p j k", j=4)
    convb, br, bi, lam = pk1[:, 40:44], pk1[:, 44:48], pk1[:, 48:52], pk1[:, 52:56]
    flag = pk1[:, 56:57]
    kdec = pk1[:, 64:72]
    kdecN = pk1[:, 72:200].rearrange("p (n h) -> p n h", n=16)
    dtab = pk2[:, 0:1024].rearrange("p (a j n) -> p a j n", a=2, j=4)
    qdec = pk2[:, 1024:1536].rearrange("p (j n) -> p j n", j=4)
    cdt = pk2[:, 1536:2048].rearrange("p (j n) -> p j n", j=4)
    maskbd = pk2[:, 2048:2560].rearrange("p (j n) -> p j n", j=4)
    ident = sb("ident", [128, 128], BF16)
    ones = sb("ones", [128, 128], BF16)
    n8sp = sb("n8sp", [128, 4])
    n16sp = sb("n16sp", [128, 4])
    n4sp = sb("n4sp", [128, 4])
    hbr = sb("hbr", [128, 4])
    hbi = sb("hbi", [128, 4])
    qtrc = sb("qtrc", [128, 1])
    epsc = sb("epsc", [128, 1])
    onec = sb("onec", [128, 1])
    wr32 = rt1.rearrange("p (j n) -> p j n", j=4)
    wi32 = rt2.rearrange("p (j n) -> p j n", j=4)
    wrbd = sb("wrbd", [128, 4, 128], BF16)
    wibd = sb("wibd", [128, 4, 128], BF16)

    banks = [nc.alloc_psum_tensor(f"ps{b}", [128, 512], F32).ap() if b != 3 else None for b in range(8)]
    pT_all = nc.alloc_psum_tensor("ps3", [128, 8, 128], BF16).ap()
    rot = [0]
    ROT = [0, 1, 2, 4, 5]

    def nextbank():
        b = ROT[rot[0] % 5]
        rot[0] += 1
        return b

    def bk(b):
        return ("ps", b)

    def dma(eng, out, in_, reads, writes):
        S.add(eng, lambda e, o=out, i=in_: e.dma_start(out=o, in_=i), reads, writes, dma=True)

    def act(out, in_, func, reads, writes, bias=None, scale=None):
        kw = {}
        if bias is not None:
            kw["bias"] = bias
        if scale is not None:
            kw["scale"] = scale
        S.add("act", lambda e, o=out, i=in_, f=func, kw=kw: e.activation(out=o, in_=i, func=f, **kw),
              reads, writes)

    def tt(eng, out, in0, in1, op, reads, writes):
        S.add(eng, lambda e, o=out, a=in0, b=in1, p=op: e.tensor_tensor(out=o, in0=a, in1=b, op=p),
              reads, writes)

    def ts(eng, out, in0, s1, op0, reads, writes, s2=None, op1=None):
        if op1 is None:
            S.add(eng, lambda e, o=out, a=in0, s=s1, p=op0: e.tensor_scalar(out=o, in0=a, scalar1=s, scalar2=None, op0=p),
                  reads, writes)
        else:
            S.add(eng, lambda e, o=out, a=in0, s=s1, p=op0, s_2=s2, p1=op1:
                  e.tensor_scalar(out=o, in0=a, scalar1=s, scalar2=s_2, op0=p, op1=p1), reads, writes)

    def stt(out, in0, scalar, in1, op0, op1, reads, writes):
        S.add("dve", lambda e, o=out, a=in0, s=scalar, b=in1, p0=op0, p1=op1:
              e.scalar_tensor_tensor(out=o, in0=a, scalar=s, in1=b, op0=p0, op1=p1), reads, writes)

    def copy(eng, out, in_, reads, writes):
        if eng == "act":
            S.add("act", lambda e, o=out, i=in_: e.copy(out=o, in_=i), reads, writes)
        else:
            S.add(eng, lambda e, o=out, i=in_: e.tensor_copy(out=o, in_=i), reads, writes)

    def mm_group(out, okey, pairs, reads):
        n = len(pairs)
        for i, pr in enumerate(pairs):
            l, r = pr[0], pr[1]
            rk = list(reads) + (list(pr[2]) if len(pr) > 2 else [])
            S.add("pe", lambda e, o=out, l=l, r=r, st=(i == 0), sp=(i == n - 1):
                  e.matmul(o, lhsT=l, rhs=r, start=st, stop=sp), rk, [okey])

    def mm1(out, okey, l, r, reads, start=True, stop=True):
        S.add("pe", lambda e, o=out, l=l, r=r, st=start, sp=stop: e.matmul(o, lhsT=l, rhs=r, start=st, stop=sp),
              reads, [okey])

    def transp(out, okey, in_, reads):
        S.add("pe", lambda e, o=out, i=in_: e.transpose(o, i, ident), list(reads) + ["ident"], [okey])

    def v3(ap, a):
        return ap.rearrange("p (a b) -> p a b", a=a)

    def blk_src(b):
        dst = scr[b]
        if b < 10:
            return [(v3(dst, 8), w_in.rearrange("(c p) n -> p c n", p=128)[:, :, b * 512:(b + 1) * 512])]
        if b < 12:
            k = b - 10
            d4 = dst.rearrange("p (s c n) -> p s c n", s=2, c=4)
            return [(d4[:, 0], w_a.rearrange("(c p) n -> p c n", p=128)[:, :, k * 512:(k + 1) * 512]),
                    (d4[:, 1], w_b.rearrange("(c p) n -> p c n", p=128)[:, :, k * 512:(k + 1) * 512])]
        if b < 14:
            k = b - 12
            return [(v3(dst, 8), w_out.rearrange("(c p) n -> p c n", p=128)[:, :, k * 512:(k + 1) * 512])]
        if b < 22:
            k = b - 14
            return [(v3(dst, 8), w1.rearrange("(c p) n -> p c n", p=128)[:, :, k * 512:(k + 1) * 512])]
        j = b - 22
        d3 = v3(dst, 32)
        s3 = w2.rearrange("(f p) n -> p f n", p=128)[:, :, j * 128:(j + 1) * 128]
        return [(d3[:, q * 8:(q + 1) * 8, :], s3[:, q * 8:(q + 1) * 8, :]) for q in range(4)]

    dbg = dbg or set()
    cast_rest = [1, 2, 5, 10, 6, 8, 11, 7, 9] + list(range(12, 30))

    def cast_block(b):
        if b >= 22:
            q = b - 22
            dma("pool", scrw2[:, q * 4:(q + 1) * 4, :],
                w2.rearrange("(f p) n -> p f n", p=128)[:, q * 4:(q + 1) * 4, :], [], [("scrw2", q)])
            return
        for (dv, sv) in blk_src(b):
            dma("pool", dv, sv, [], [("scr", b)])

    def cast_quantum():
        if cast_rest:
            cast_block(cast_rest.pop(0))

    for b in (0, 3, 4):
        cast_block(b)

    ring_free = [0, 1, 2, 3]

    def wload(b):
        s = ring_free.pop(0)
        if b >= 22:
            j = b - 22
            dma("sp", wring[:, s, :].rearrange("p (f n) -> p f n", f=32), scrw2[:, :, j * 128:(j + 1) * 128],
                [("scrw2", q) for q in range(8)], [("w", s)])
        else:
            dma("sp", wring[:, s, :], scr[b], [("scr", b)], [("w", s)])
        return wring[:, s, :], ("w", s), s

    def wfree(s):
        ring_free.append(s)

    dma("sp", xT[0], xT_d.rearrange("(c p) t -> p c t", p=128)[:, :, 0:T], [], [("xT", 0, c) for c in range(8)])
    dma("sp", cos_s[0], cos_d[:, 0:4, :], [], [("cos", 0)])
    dma("sp", sin_s[0], sin_d[:, 0:4, :], [], [("sin", 0)])
    dma("sp", pk1, pk1_d, [], ["g1", "g2", "gf", "convw", "convb", "br", "bi", "lam", "flag", "kdec", "kdecN"])
    dma("sp", wr32, wr_d, [], ["rt1"])
    dma("sp", wi32, wi_d, [], ["rt2"])
    dma("sp", pk2, pk2_d, [], ["dtab", "qdec", "cdt", "maskbd"])
    dma("pool", ident, ident_d, [], ["ident"])
    S.add("dve", lambda e: e.memset(ones, 1.0 / 1024.0), [], ["ones"])
    S.add("dve", lambda e: e.memset(epsc, EPS), [], ["epsc"])
    S.add("dve", lambda e: e.memset(onec, 1.0), [], ["onec"])
    S.add("dve", lambda e: e.memset(hist, 0.0), [], ["hist"])
    S.add("dve", lambda e: e.memset(hstate, 0.0), [], ["hstate"])
    S.add("dve", lambda e: e.memset(st32, 0.0), [], ["st32"])
    S.add("dve", lambda e: e.memset(stbf[0], 0.0), [], [("stbf", 0)])
    copy("dve", wrbd, wr32, ["rt1"], ["wrbd"])
    copy("dve", wibd, wi32, ["rt2"], ["wibd"])
    act(n8sp, lam, AF.Exp, ["lam"], ["n8sp"], scale=-1.0)
    act(n8sp, n8sp, AF.Ln, ["n8sp", "onec"], ["n8sp"], bias=onec[:, 0:1], scale=1.0)
    ts("dve", n16sp, n8sp, -16.0, ALU.mult, ["n8sp"], ["n16sp"])
    ts("dve", n4sp, n8sp, -4.0, ALU.mult, ["n8sp"], ["n4sp"])
    ts("dve", n8sp, n8sp, -8.0, ALU.mult, ["n8sp", "n16sp", "n4sp"], ["n8sp"])
    ts("dve", hbr, br, 0.5, ALU.mult, ["br"], ["hbr"])
    ts("dve", hbi, bi, 0.5, ALU.mult, ["bi"], ["hbi"])
    S.add("dve", lambda e: e.memset(qtrc, 0.25), [], ["qtrc"])

    def xkeys(xb):
        return [("xT", xb, c) for c in range(8)]

    def hkeys():
        return [("hT", c) for c in range(8)]

    def load_x(st_idx):
        xb = st_idx % 2
        src = xT_d.rearrange("(c p) t -> p c t", p=128)[:, :, st_idx * T:(st_idx + 1) * T]
        dma("sp", xT[xb], src, [], xkeys(xb))
        cb = st_idx % 2
        dma("sp", cos_s[cb], cos_d[:, st_idx * 4:(st_idx + 1) * 4, :], [], [("cos", cb)])
        dma("sp", sin_s[cb], sin_d[:, st_idx * 4:(st_idx + 1) * 4, :], [], [("sin", cb)])

    def norm_stats_chunk(xb, c, sbank):
        act(sqb[:, c, :], xT[xb][:, c, :], AF.Square, [("xT", xb, c)], [("sq", c)])
        S.add("pe", lambda e, c=c, sbank=sbank: e.matmul(banks[sbank], lhsT=ones, rhs=sqb[:, c, :],
                                                         start=(c == 0), stop=(c == 7)),
              ["ones", ("sq", c)], [bk(sbank)])

    def norm_finish(sbank, w):
        act(rstd2[w], banks[sbank], AF.Ln, [bk(sbank), "epsc"], [("rstd", w)], bias=epsc[:, 0:1], scale=1.0)
        act(rstd2[w], rstd2[w], AF.Exp, [("rstd", w)], [("rstd", w)], scale=-0.5)

    def norm_apply(xb, gcol, gkey, w, to_x, after=None):
        for c in range(8):
            xk = ("xT", xb, c)
            if to_x:
                stt(xT[xb][:, c, :], xT[xb][:, c, :], gcol[:, c:c + 1], rstd2[w], ALU.mult, ALU.mult,
                    [xk, gkey, ("rstd", w)], [xk])
                if after is not None:
                    after(c)
            else:
                stt(hT[:, c, :], xT[xb][:, c, :], gcol[:, c:c + 1], rstd2[w], ALU.mult, ALU.mult,
                    [xk, gkey, ("rstd", w)], [("hT", c)])

    def norm1_stats(st_idx):
        xb = st_idx % 2
        for c in range(8):
            norm_stats_chunk(xb, c, 6)
        norm_finish(6, 1)

    tokbank = [0]

    def tok_unit(nm, t, wb_, kw, main, cb, st_idx, bpair=(5, 7)):
        pb = bpair[tokbank[0] % len(bpair)]
        tokbank[0] += 1
        mm_group(banks[pb], bk(pb), [(hT[:, c, t * 128:(t + 1) * 128], wb_[:, c, :], [("hT", c)]) for c in range(8)],
                 [kw])
        P3 = banks[pb].rearrange("p (h d) -> p h d", h=8)
        if nm in ("q", "k"):
            dst = q_r[t] if nm == "q" else k_r[t]
            dkey = ("q_r", t) if nm == "q" else ("k_r", t)
            cs = cos_s[cb][:, t, :].unsqueeze(1).broadcast_to([128, 8, 64])
            s_lo = sin_s[cb][:, t, 0:32].unsqueeze(1).broadcast_to([128, 8, 32])
            s_hi = sin_s[cb][:, t, 32:64].unsqueeze(1).broadcast_to([128, 8, 32])
            r1 = rt1.rearrange("p (h d) -> p h d", h=8)
            r2 = rt2.rearrange("p (h d) -> p h d", h=8)
            tt("dve", r1, P3, cs, ALU.mult, [bk(pb), ("cos", cb)], ["rt1"])
            tt("dve", r2[:, :, 0:32], P3[:, :, 32:64], s_lo, ALU.mult, [bk(pb), ("sin", cb)], ["rt2"])
            tt("dve", r2[:, :, 32:64], P3[:, :, 0:32], s_hi, ALU.mult, [bk(pb), ("sin", cb)], ["rt2"])
            tt("pool", dst, rt1, rt2, ALU.add, ["rt1", "rt2"], [dkey])
            if nm == "k":
                if main:
                    kd3 = kdec.unsqueeze(2).broadcast_to([128, 8, 64])
                else:
                    kd3 = kdecN[:, st_idx * 4 + t, :].unsqueeze(2).broadcast_to([128, 8, 64])
                tt("pool", kdc[t].rearrange("p (h d) -> p h d", h=8),
                   k_r[t].rearrange("p (h d) -> p h d", h=8), kd3, ALU.mult,
                   [("k_r", t), "kdec", "kdecN"], [("kdc", t)])
        elif nm == "v":
            copy("act", v_t[t], banks[pb], [bk(pb)], [("v_t", t)])
        else:
            act(sgr[t], banks[pb], AF.Silu, [bk(pb)], [("sgr", t)])

    def lru_pieces(main, st_idx):
        W = {}

        def load():
            w_, k_, sl = wload(0)
            W["xa"] = (v3(w_, 8), k_, sl)
            if main:
                w_, k_, sl = wload(1)
                W["ga"] = (v3(w_, 8), k_, sl)

        def A(j):
            jb = j % 2
            wxa, kxa, _ = W["xa"]
            mm_group(banks[0], bk(0), [(wxa[:, c, j * 128:(j + 1) * 128], hT[:, c, :], [("hT", c)]) for c in range(8)],
                     [kxa])
            copy("act", xa_ext[jb][:, 3:T + 3], banks[0], [bk(0)], [("xa_ext", jb)])
            copy("pool", xa_ext[jb][:, 0:3], hist[:, j, :], ["hist"], [("xa_ext", jb)])
            ts("dve", xc[jb], xa_ext[jb][:, 0:T], convw[:, j, 0:1], ALU.mult,
               [("xa_ext", jb), "convw", "convb"], [("xc", jb)], s2=convb[:, j:j + 1], op1=ALU.add)
            for k in range(1, 4):
                stt(xc[jb], xa_ext[jb][:, k:k + T], convw[:, j, k:k + 1], xc[jb], ALU.mult, ALU.add,
                    [("xa_ext", jb), "convw", ("xc", jb)], [("xc", jb)])
            copy("pool", hist[:, j, :], xa_ext[jb][:, T:T + 3], [("xa_ext", jb)], ["hist"])
            copy("dve", xcb[jb], xc[jb], [("xc", jb)], [("xcb", jb)])
            if main:
                wga, kga, _ = W["ga"]
                mm_group(banks[4], bk(4), [(wga[:, c, j * 128:(j + 1) * 128], hT[:, c, :], [("hT", c)]) for c in range(8)],
                         [kga])
                act(gg[jb], banks[4], AF.Gelu_apprx_tanh, [bk(4)], [("gg", jb)])
            if j == 3:
                wfree(W["xa"][2])
                if main:
                    wfree(W["ga"][2])

        def B(j):
            jb = j % 2
            mm1(banks[2], bk(2), wrbd[:, j, :], xcb[jb], ["wrbd", ("xcb", jb)])
            mm1(banks[6], bk(6), wibd[:, j, :], xcb[jb], ["wibd", ("xcb", jb)])
            act(L2[jb], banks[2], AF.Tanh, [bk(2), "hbr"], [("L2", jb)], bias=hbr[:, j:j + 1], scale=0.5)
            act(L3[jb], banks[6], AF.Tanh, [bk(6), "hbi"], [("L3", jb)], bias=hbi[:, j:j + 1], scale=0.5)
            act(L4[jb], L2[jb], AF.Exp, [("L2", jb), "n4sp"], [("L4", jb)], bias=n4sp[:, j:j + 1],
                scale=n4sp[:, j:j + 1])
            act(L2[jb], L2[jb], AF.Exp, [("L2", jb), "n8sp"], [("L2", jb)], bias=n8sp[:, j:j + 1],
                scale=n8sp[:, j:j + 1])
            act(L2[jb], L2[jb], AF.Sqrt, [("L2", jb), "qtrc"], [("L2", jb)], bias=qtrc[:, 0:1], scale=-0.25)
            stt(L3[jb], L3[jb], 1.0, xc[jb], ALU.add, ALU.mult, [("L3", jb), ("xc", jb)], [("L3", jb)])
            tt("pool", L3[jb], L3[jb], L2[jb], ALU.mult, [("L3", jb), ("L2", jb)], [("L3", jb)])
            S.add("dve", lambda e, jb=jb, j=j: e.tensor_tensor_scan(
                out=L5[jb], data0=L4[jb], data1=L3[jb], initial=hstate[:, j:j + 1], op0=ALU.mult, op1=ALU.add),
                [("L4", jb), ("L3", jb), "hstate"], [("L5", jb)])
            copy("pool", hstate[:, j:j + 1], L5[jb][:, T - 1:T], [("L5", jb)], ["hstate"])
            if main:
                tt("pool", yaT[:, j, :], gg[jb], L5[jb], ALU.mult, [("gg", jb), ("L5", jb)], ["yaT"])
                if st_idx == NST:
                    cast_quantum()

        return load, A, B

    def tok_blocks(names):
        bid = {"q": 2, "k": 3, "v": 4, "gr": 5}
        out = []
        for nm in names:
            w_, k_, sl = wload(bid[nm])
            out.append((nm, v3(w_, 8), k_, sl))
        return out

    def lru_standalone(main, st_idx, cb):
        load, A, B = lru_pieces(main, st_idx)
        load()
        fw = tok_blocks(("q", "v") if main else ("k", "v"))
        for j in range(5):
            if j < 4:
                A(j)
                for (nm, w_, k_, sl) in fw:
                    tok_unit(nm, j, w_, k_, main, cb, st_idx, bpair=(5, 7) if main else (5, 7, 4, 1))
            if j >= 1:
                B(j - 1)
        for (nm, w_, k_, sl) in fw:
            wfree(sl)
        if main:
            rest = tok_blocks(("k", "gr"))
            for t in range(4):
                for (nm, w_, k_, sl) in rest:
                    tok_unit(nm, t, w_, k_, main, cb, st_idx)
            for (nm, w_, k_, sl) in rest:
                wfree(sl)
        else:
            cast_quantum()
            cast_quantum()
            cast_quantum()

    PEND = []

    def tok_phase(st_idx, cb):
        blks = tok_blocks(("q", "v", "k", "gr"))
        for t in range(4):
            for (nm, w_, k_, sl) in blks:
                tok_unit(nm, t, w_, k_, True, cb, st_idx, bpair=(0, 1, 2))
            if t == 0 and PEND:
                PEND.pop(0)()
            if t >= 1:
                ret_A(t - 1, (t - 1) % 2)
            if t >= 2:
                ret_B(t - 2, (t - 2) % 2)
        for (nm, w_, k_, sl) in blks:
            wfree(sl)
        ret_A(3, 1)
        ret_B(2, 0)
        ret_B(3, 1)

    stv = [0]

    def kv_update(t):
        cur = stv[0] % 2
        nxt = 1 - cur
        kvp = banks[7].rearrange("p (j n) -> p j n", j=4)
        tt("pool", st32, st32, cdt, ALU.mult, ["st32", "cdt"], ["st32"])
        for j in range(4):
            mm1(kvp[:, j, :], bk(7), kdc[t][:, j * 128:(j + 1) * 128], v_t[t][:, j * 128:(j + 1) * 128],
                [("kdc", t), ("v_t", t)])
        tt("dve", kvt, kvp, maskbd, ALU.mult, [bk(7), "maskbd"], ["kvt"])
        tt("dve", st32, st32, kvt, ALU.add, ["st32", "kvt"], ["st32"])
        copy("act", stbf[nxt], st32, ["st32"], [("stbf", nxt)])
        stv[0] += 1

    def kv_prepass():
        kvp = banks[7].rearrange("p (j n) -> p j n", j=4)
        for j in range(4):
            for t in range(4):
                mm1(kvp[:, j, :], bk(7), kdc[t][:, j * 128:(j + 1) * 128], v_t[t][:, j * 128:(j + 1) * 128],
                    [("kdc", t), ("v_t", t)], start=(t == 0), stop=(t == 3))
        tt("dve", kvt, kvp, maskbd, ALU.mult, [bk(7), "maskbd"], ["kvt"])
        tt("dve", st32, st32, kvt, ALU.add, ["st32", "kvt"], ["st32"])

    def ret_A(t, sci):
        pT = pT_all
        for j in range(4):
            transp(pT[:, j, :], bk(3), q_r[t][:, j * 128:(j + 1) * 128], [("q_r", t)])
        for j in range(4):
            transp(pT[:, 4 + j, :], bk(3), k_r[t][:, j * 128:(j + 1) * 128], [("k_r", t)])
        copy("act", qTs, pT[:, 0:4, :], [bk(3)], ["qTs"])
        copy("act", kTs, pT[:, 4:8, :], [bk(3)], ["kTs"])
        tt("dve", qTd[sci], pT[:, 0:4, :], qdec, ALU.mult, [bk(3), "qdec"], [("qTd", sci)])
        for h in range(8):
            par, j = h % 2, h // 2
            o = par * 64
            mm1(banks[4 + par][:, j * 128:(j + 1) * 128], bk(4 + par), kTs[o:o + 64, j, :], qTs[o:o + 64, j, :],
                ["kTs", "qTs"])
        sc = scT[sci]
        for par in range(2):
            tt("dve", sc[:, par].rearrange("p j n -> p (j n)"), banks[4 + par],
               dtab[:, par].rearrange("p j n -> p (j n)"), ALU.mult, [bk(4 + par), "dtab"], [("scT", sci)])

    def ret_B(t, sci):
        cur = stv[0] % 2
        pT = pT_all
        sc = scT[sci]
        for j in range(4):
            mm1(banks[6][:, j * 128:(j + 1) * 128], bk(6), qTd[sci][:, j, :], stbf[cur][:, j, :],
                [("qTd", sci), ("stbf", cur)], start=True, stop=False)
            for par in range(2):
                h = 2 * j + par
                mm1(banks[6][:, h * 64:(h + 1) * 64], bk(6), sc[:, par, j, :], v_t[t][:, h * 64:(h + 1) * 64],
                    [("scT", sci), ("v_t", t)], start=False, stop=(par == 1))
        kv_update(t)
        act(sqr, banks[6], AF.Square, [bk(6)], ["sqr"])
        S.add("dve", lambda e: e.tensor_reduce(out=ssq, in_=sqr.rearrange("p (h d) -> p h d", h=8),
                                               op=ALU.add, axis=AX.X), ["sqr"], ["ssq"])
        act(ssq, ssq, AF.Ln, ["ssq", "epsc"], ["ssq"], bias=epsc[:, 0:1], scale=1.0 / 64.0)
        act(ssq, ssq, AF.Exp, ["ssq"], ["ssq"], scale=-0.5)
        tt("dve", tmp2.rearrange("p (h d) -> p h d", h=8), banks[6].rearrange("p (h d) -> p h d", h=8),
           ssq.unsqueeze(2).broadcast_to([128, 8, 64]), ALU.mult, [bk(6), "ssq"], ["tmp2"])
        tt("pool", ybt, tmp2, sgr[t], ALU.mult, ["tmp2", ("sgr", t)], ["ybt"])
        for j in range(4):
            transp(pT[:, j, :], bk(3), ybt[:, j * 128:(j + 1) * 128], ["ybt"])
        copy("act", ybT[:, :, t * 128:(t + 1) * 128], pT[:, 0:4, :], [bk(3)], ["ybT"])

    def retention():
        for f, t, b in ((ret_A, 0, 0), (ret_A, 1, 1), (ret_B, 0, 0), (ret_A, 2, 0), (ret_B, 1, 1), (ret_A, 3, 1),
                        (ret_B, 2, 0), (ret_B, 3, 1)):
            f(t, b)
            cast_quantum()

    def merge(xb):
        for half in range(2):
            wab, kab, sl_ab = wload(10 + half)
            wab = wab.rearrange("p (s c n) -> p s c n", s=2, c=4)
            wsa, ksa, sl_sa = wload(6 + half)
            wsa = v3(wsa, 8)
            wsb, ksb, sl_sb = wload(8 + half)
            wsb = v3(wsb, 8)
            for jj in range(4):
                j = half * 4 + jj
                jb = j % 2
                cs = slice(jj * 128, (jj + 1) * 128)
                bsa, bsb, bpa, bpb = nextbank(), nextbank(), nextbank(), nextbank()
                mm_group(banks[bsa], bk(bsa), [(wsa[:, c, cs], hT[:, c, :], [("hT", c)]) for c in range(8)], [ksa])
                mm_group(banks[bsb], bk(bsb), [(wsb[:, c, cs], hT[:, c, :], [("hT", c)]) for c in range(8)], [ksb])
                mm_group(banks[bpa], bk(bpa), [(wab[:, 0, c, cs], yaT[:, c, :]) for c in range(4)], [kab, "yaT"])
                mm_group(banks[bpb], bk(bpb), [(wab[:, 1, c, cs], ybT[:, c, :]) for c in range(4)], [kab, "ybT"])
                act(siga[jb], banks[bsa], AF.Sigmoid, [bk(bsa)], [("siga", jb)])
                act(sigb[jb], banks[bsb], AF.Sigmoid, [bk(bsb)], [("sigb", jb)])
                tt("dve", siga[jb], banks[bpa], siga[jb], ALU.mult, [bk(bpa), ("siga", jb)], [("siga", jb)])
                tt("dve", sigb[jb], banks[bpb], sigb[jb], ALU.mult, [bk(bpb), ("sigb", jb)], [("sigb", jb)])
                tt("pool", mT[:, j, :], siga[jb], sigb[jb], ALU.add, [("siga", jb), ("sigb", jb)], [("mT", j)])
            wfree(sl_ab)
            wfree(sl_sa)
            wfree(sl_sb)
            cast_quantum()
            cast_quantum()
        for half in range(2):
            wo, ko, sl_o = wload(12 + half)
            wo = v3(wo, 8)
            for jj in range(4):
                j = half * 4 + jj
                pb = nextbank()
                mm_group(banks[pb], bk(pb), [(wo[:, c, jj * 128:(jj + 1) * 128], mT[:, c, :], [("mT", c)]) for c in range(8)],
                         [ko])
                tt("dve", xT[xb][:, j, :], banks[pb], xT[xb][:, j, :], ALU.add, [bk(pb), ("xT", xb, j)],
                   [("xT", xb, j)])
                if j >= 1:
                    norm_stats_chunk(xb, j - 1, 7)
            wfree(sl_o)
        norm_stats_chunk(xb, 7, 7)
        norm_finish(7, 0)

    LRU_SCHED = [[("A", 0)], [("A", 1)], [("B", 0)], [("A", 2)], [("B", 1)], [("A", 3)], [("B", 2)], [("B", 3)]]

    def ffn(xb, st_idx):
        n = 0
        for b in range(8):
            wv, kw, sl = wload(14 + b)
            wv = v3(wv, 8)
            for i in range(4):
                pb = nextbank()
                rb = n % 2
                n += 1
                mm_group(banks[pb], bk(pb), [(wv[:, c, i * 128:(i + 1) * 128], hT[:, c, :], [("hT", c)]) for c in range(8)],
                         [kw])
                act(rl[rb], banks[pb], AF.Relu, [bk(pb)], [("rl", rb)])
                tt("pool", hidT[:, b * 4 + i, :], rl[rb], rl[rb], ALU.mult, [("rl", rb)], [("hid", b * 4 + i)])
            wfree(sl)
            if b == 4 and st_idx + 1 < n_st:
                norm1_stats(st_idx + 1)
        overlap = st_idx + 1 < n_st
        if overlap:
            norm_apply((st_idx + 1) % 2, g1, "g1", 1, to_x=False)
            load, A, B = lru_pieces(True, st_idx + 1)
        for j in range(8):
            wv, kw, sl = wload(22 + j)
            wv = v3(wv, 32)
            pb = (1, 5)[j % 2]
            mm_group(banks[pb], bk(pb), [(wv[:, f, :], hidT[:, f, :], [("hid", f)]) for f in range(32)], [kw])
            wfree(sl)
            tt("dve", xT[xb][:, j, :], banks[pb], xT[xb][:, j, :], ALU.add, [bk(pb), ("xT", xb, j)],
               [("xT", xb, j)])
            if j >= 1:
                norm_stats_chunk(xb, j - 1, 7)
            if overlap:
                if j == 0:
                    load()
                for kind, jj in LRU_SCHED[j]:
                    (A if kind == "A" else B)(jj)
        norm_stats_chunk(xb, 7, 7)
        norm_finish(7, 0)

    sci = [0]
    norm1_stats(0)
    for st_idx in range(n_st):
        main = st_idx >= NST
        xb = st_idx % 2
        cb = st_idx % 2
        if st_idx == NST:
            while cast_rest and cast_rest[0] < 14:
                cast_quantum()
            ts("dve", hstate, hstate, flag[:, 0:1], ALU.mult, ["hstate", "flag"], ["hstate"])
        if st_idx == 0:
            norm_apply(xb, g1, "g1", 1, to_x=False)
        if st_idx <= NST:
            lru_standalone(main, st_idx, cb)
        else:
            tok_phase(st_idx, cb)
        if st_idx + 1 < n_st:
            load_x(st_idx + 1)
        if main:
            if st_idx == NST:
                retention()
            merge(xb)
            norm_apply(xb, g2, "g2", 0, to_x=False)
            while cast_rest:
                cast_quantum()
            ffn(xb, st_idx)
            m = st_idx - NST
            dst = outT_d.rearrange("(c p) t -> p c t", p=128)[:, :, m * T:(m + 1) * T]
            if st_idx == n_st - 1:
                norm_apply(xb, gf, "gf", 0, to_x=True,
                           after=lambda c: dma("act", dst[:, c, :], xT[xb][:, c, :], [("xT", xb, c)], [("out", m, c)]))
            else:
                norm_apply(xb, gf, "gf", 0, to_x=True)
                PEND.append(lambda dst=dst, xb=xb, m=m: dma("act", dst, xT[xb], xkeys(xb), [("out", m)]))
        else:
            if st_idx + 1 < n_st:
                norm1_stats(st_idx + 1)
                norm_apply((st_idx + 1) % 2, g1, "g1", 1, to_x=False)
            kv_prepass()
            if st_idx == NST - 1:
                copy("act", stbf[stv[0] % 2], st32, ["st32"], [("stbf", stv[0] % 2)])
        S.new_epoch()
    S.emit()
    return nc


def _const_tables():
    nh, dh, C = 8, 64, 128
    log_g = np.log1p(-np.exp2(-5.0 - np.arange(nh, dtype=np.float64)))
    pos = np.arange(C, dtype=np.float64)
    rel = pos[None, :] - pos[:, None]
    dtab = np.zeros((C, 2, 4, C), np.float64)
    for h in range(nh):
        dtab[:, h % 2, h // 2, :] = np.where(rel >= 0, np.exp(log_g[h] * np.maximum(rel, 0.0)), 0.0) * 0.125
    qdec = np.zeros((128, 4, C), np.float64)
    cdt = np.zeros((128, 4, 128), np.float64)
    mask = np.zeros((128, 4, 128), np.float64)
    for p in range(128):
        for j in range(4):
            h = 2 * j + p // 64
            qdec[p, j, :] = np.exp(log_g[h] * (pos + 1.0))
            cdt[p, j, :] = np.exp(log_g[h] * C)
            mask[p, j, (p // 64) * 64:(p // 64) * 64 + 64] = 1.0
    kdec = np.zeros((C, nh), np.float64)
    for h in range(nh):
        kdec[:, h] = np.exp(log_g[h] * (C - 1.0 - pos)) * 0.125
    kdecN = np.zeros((C, 16, nh), np.float64)
    for n in range(16):
        for h in range(nh):
            kdecN[:, n, h] = np.exp(log_g[h] * (C - 1.0 - pos + C * (15 - n))) * 0.125
    ident = np.eye(128, dtype=np.float32)
    return (dtab.astype(np.float32), qdec.astype(np.float32), kdec.astype(np.float32),
            cdt.astype(np.float32), mask.astype(np.float32), ident, kdecN.astype(np.float32))


def _rope_tables(pos0):
    half = 32
    inv = 10000.0 ** (-np.arange(half, dtype=np.float64) / half)
    p = np.arange(128)[:, None] + np.arange(32)[None, :] * 128 + pos0
    ang = p.astype(np.float64)[:, :, None] * inv[None, None, :]
    c, s = np.cos(ang), np.sin(ang)
    cos2 = np.concatenate([c, c], axis=-1).astype(np.float32)
    sin2 = np.concatenate([-s, s], axis=-1).astype(np.float32)
    return np.ascontiguousarray(cos2), np.ascontiguousarray(sin2)


def kernel(x, norm1_g, w_in, conv_w, conv_b, lru_wr, lru_br, lru_wi, lru_bi, lru_lambda,
           w_branch_a, w_branch_b, w_out, norm2_g, w_ff1, w_ff2, norm_f_g):
    f32 = np.float32
    x = np.asarray(x, f32)

    def pc(v, n):
        return np.ascontiguousarray(np.asarray(v, f32).reshape(n, 128).T)

    dtab, qdec, kdec, cdt, mask, ident, kdecN = _const_tables()
    shared = {
        "w_in": np.ascontiguousarray(np.asarray(w_in, f32)[0]),
        "w_a": np.ascontiguousarray(np.asarray(w_branch_a, f32)[0]),
        "w_b": np.ascontiguousarray(np.asarray(w_branch_b, f32)[0]),
        "w_out": np.ascontiguousarray(np.asarray(w_out, f32)[0]),
        "w1": np.ascontiguousarray(np.asarray(w_ff1, f32)[0]),
        "w2": np.ascontiguousarray(np.asarray(w_ff2, f32)[0]),
    }
    wr_bd = np.zeros((128, 4, 128), f32)
    wi_bd = np.zeros((128, 4, 128), f32)
    wr0, wi0 = np.asarray(lru_wr, f32)[0], np.asarray(lru_wi, f32)[0]
    for blk in range(8):
        j, o = blk // 2, (blk % 2) * 64
        wr_bd[o:o + 64, j, o:o + 64] = wr0[blk]
        wi_bd[o:o + 64, j, o:o + 64] = wi0[blk]
    shared["wr"] = wr_bd
    shared["wi"] = wi_bd
    shared["ident"] = ident
    pk2 = np.concatenate([dtab.reshape(128, -1), qdec.reshape(128, -1), cdt.reshape(128, -1),
                          mask.reshape(128, -1)], axis=1).astype(f32)
    shared["pk2"] = np.ascontiguousarray(pk2)

    def pack1(flagval):
        pk = np.zeros((128, PK1), f32)
        pk[:, 0:8] = pc(np.asarray(norm1_g)[0], 8)
        pk[:, 8:16] = pc(np.asarray(norm2_g)[0], 8)
        pk[:, 16:24] = pc(norm_f_g, 8)
        pk[:, 24:40] = np.asarray(conv_w, f32)[0].reshape(4, 4, 128).transpose(2, 1, 0).reshape(128, 16)
        pk[:, 40:44] = pc(np.asarray(conv_b)[0], 4)
        pk[:, 44:48] = pc(np.asarray(lru_br)[0].reshape(-1), 4)
        pk[:, 48:52] = pc(np.asarray(lru_bi)[0].reshape(-1), 4)
        pk[:, 52:56] = pc(np.asarray(lru_lambda)[0], 4)
        pk[:, 56] = flagval
        pk[:, 64:72] = kdec
        pk[:, 72:200] = kdecN.reshape(128, 128)
        return pk

    in_maps = []
    for c in range(8):
        b, half = c // 2, c % 2
        xt = np.zeros((D, 2 * TOK), f32)
        if half == 1:
            xt[:, :TOK] = x[b, :TOK].T
        xt[:, TOK:] = x[b, half * TOK:(half + 1) * TOK].T
        cos2, sin2 = _rope_tables(0 if half == 1 else -TOK)
        m = dict(shared)
        m["xT"] = xt
        m["pk1"] = pack1(float(half))
        m["cos2"] = cos2
        m["sin2"] = sin2
        in_maps.append(m)
    nc = build_nc()
    res = run_bass_kernel_spmd(nc, in_maps, core_ids=list(range(8)))
    out = np.empty((NB, SEQ, D), f32)
    for c in range(8):
        b, half = c // 2, c % 2
        out[b, half * TOK:(half + 1) * TOK, :] = np.asarray(res.results[c]["outT"], f32).T
    return out
```

```python
import contextlib
import numpy as np
import concourse.bass as bass
import concourse.mybir as mybir
from concourse.bass_utils import run_bass_kernel_spmd

F32 = mybir.dt.float32
BF16 = mybir.dt.bfloat16
AF = mybir.ActivationFunctionType
ALU = mybir.AluOpType
AX = mybir.AxisListType

D = 1024
SEQ = 4096
NB = 4
TOK = 2048
T = 512
NST = TOK // T
EPS = 1e-6
NBLK = 30
PK1 = 200
PK2 = 2560


class _Op:
    __slots__ = ("eng", "fn", "deps", "idx", "is_dma", "sem", "semval", "milestone",
                 "mnum", "epoch", "prewait")


class Sched:
    ENGS = ("pe", "act", "dve", "pool", "sp")
    DMA_KS = {"pe": 8, "act": 8, "dve": 8, "pool": 48, "sp": 8}

    def __init__(self, nc):
        self.nc = nc
        self.ops = {e: [] for e in self.ENGS}
        self.lastw = {}
        self.readers = {}
        self.epoch = 0
        self.dma_n = {e: 0 for e in self.ENGS}
        self.alias = {}
        self.bank_last = {}

    def add_alias(self, a, b):
        self.alias.setdefault(a, []).append(b)
        self.alias.setdefault(b, []).append(a)

    def new_epoch(self):
        self.epoch += 1

    def add(self, eng, fn, reads=(), writes=(), dma=False):
        op = _Op()
        op.eng = eng
        op.fn = fn
        op.is_dma = dma
        op.milestone = False
        op.mnum = 0
        op.epoch = self.epoch
        op.sem = None
        op.semval = 0
        op.prewait = None
        deps = {}
        for k in reads:
            w = self.lastw.get(k)
            if w is not None:
                deps[id(w)] = (w, True)
        for k in list(reads) + list(writes):
            if isinstance(k, tuple) and k and k[0] == "ps":
                bl = self.bank_last.setdefault(k, {})
                for e2, o2 in bl.items():
                    if e2 != eng and id(o2) not in deps:
                        deps[id(o2)] = (o2, False)
                bl[eng] = op
        if self.alias:
            wl = list(writes)
            for k in writes:
                for a in self.alias.get(k, ()):
                    if a not in wl:
                        wl.append(a)
            writes = wl
        for k in writes:
            w = self.lastw.get(k)
            if w is not None and id(w) not in deps:
                deps[id(w)] = (w, False)
            rd = self.readers.get(k)
            if rd:
                for r in rd[0].values():
                    if id(r) not in deps:
                        deps[id(r)] = (r, False)
                for r in rd[1]:
                    if id(r) not in deps:
                        deps[id(r)] = (r, False)
        for k in reads:
            rd = self.readers.get(k)
            if rd is None:
                rd = self.readers[k] = ({}, [])
            if dma:
                rd[1].append(op)
            else:
                rd[0][eng] = op
        for k in writes:
            self.lastw[k] = op
            self.readers[k] = ({}, [])
        fin = []
        for (d, raw) in deps.values():
            if d is op:
                continue
            if (not d.is_dma) and d.eng == eng and not dma:
                if eng == "pe":
                    continue
            fin.append(d)
        op.deps = fin
        lst = self.ops[eng]
        op.idx = len(lst)
        lst.append(op)
        if dma:
            i = self.dma_n[eng]
            self.dma_n[eng] = i + 1
            K = self.DMA_KS[eng]
            op.sem = (eng, i % K)
            op.semval = 16 * (i // K + 1)
            if i >= K:
                op.prewait = (op.sem, 16 * (i // K))
        return op

    def emit(self):
        nc = self.nc
        for e in self.ENGS:
            for op in self.ops[e]:
                for d in op.deps:
                    if not d.is_dma:
                        d.milestone = True
        nep = self.epoch + 1
        for e in self.ENGS:
            cnt = [0] * nep
            for op in self.ops[e]:
                if op.is_dma:
                    continue
                if op.milestone:
                    cnt[op.epoch] += 1
                    op.mnum = cnt[op.epoch]
        stack = contextlib.ExitStack()
        sems = {}
        with stack:
            for e in self.ENGS:
                for p in range(nep):
                    if any((not o.is_dma) and o.milestone and o.epoch == p for o in self.ops[e]):
                        sems[(e, p)] = stack.enter_context(nc.semaphore(f"s_{e}_{p}"))
                if self.dma_n[e]:
                    for k in range(min(self.DMA_KS[e], self.dma_n[e])):
                        sems[("dma", e, k)] = stack.enter_context(nc.semaphore(f"d_{e}_{k}"))
            block = stack.enter_context(nc.Block())
            hooks = {"pe": block.tensor, "act": block.scalar, "dve": block.vector,
                     "pool": block.gpsimd, "sp": block.sync}
            all_dma_last = {}
            for e in self.ENGS:
                for op in self.ops[e]:
                    if op.is_dma:
                        all_dma_last[("dma", e, op.sem[1])] = op.semval

            def mk(e):
                def body(eng):
                    waited = {}

                    def wait(key, val):
                        if waited.get(key, 0) >= val:
                            return
                        waited[key] = val
                        eng.wait_ge(sems[key], val)

                    for op in self.ops[e]:
                        for d in op.deps:
                            if d.is_dma:
                                wait(("dma", d.sem[0], d.sem[1]), d.semval)
                            else:
                                wait((d.eng, d.epoch), d.mnum)
                        if op.prewait is not None:
                            wait(("dma", op.prewait[0][0], op.prewait[0][1]), op.prewait[1])
                        ins = op.fn(eng)
                        if op.is_dma:
                            ins.then_inc(sems[("dma", op.sem[0], op.sem[1])], 16)
                        elif op.milestone:
                            ins.then_inc(sems[(e, op.epoch)], 1)
                    if e == "sp":
                        for key, val in all_dma_last.items():
                            wait(key, val)
                return body

            for e in self.ENGS:
                if self.ops[e] or e == "sp":
                    hooks[e](mk(e))


def build_nc(n_st=2 * NST, dbg=None):
    nc = bass.Bass("TRN2", target_bir_lowering=False)

    def din(name, shape, dt=F32):
        return nc.dram_tensor(name, list(shape), dt, kind="ExternalInput").ap()

    xT_d = din("xT", [D, 2 * TOK])
    w_in = din("w_in", [D, 5120])
    w_a = din("w_a", [512, D])
    w_b = din("w_b", [512, D])
    w_out = din("w_out", [D, D])
    w1 = din("w1", [D, 4096])
    w2 = din("w2", [4096, D])
    wr_d = din("wr", [128, 4, 128])
    wi_d = din("wi", [128, 4, 128])
    pk1_d = din("pk1", [128, PK1])
    pk2_d = din("pk2", [128, PK2])
    cos_d = din("cos2", [128, 32, 64])
    sin_d = din("sin2", [128, 32, 64])
    ident_d = din("ident", [128, 128])
    outT_d = nc.dram_tensor("outT", [D, TOK], F32, kind="ExternalOutput").ap()
    scr = nc.dram_tensor("scr", [22, 128, 4096], BF16, kind="Internal").ap()
    scrw2 = nc.dram_tensor("scrw2", [128, 32, 1024], BF16, kind="Internal").ap()

    S = Sched(nc)

    def sb(name, shape, dt=F32):
        return nc.alloc_sbuf_tensor("sb_" + name, list(shape), dt).ap()

    xT = [sb(f"xT{i}", [128, 8, T]) for i in range(2)]
    hT = sb("hT", [128, 8, T], BF16)
    rstd2 = [sb(f"rstd{i}", [128, T]) for i in range(2)]
    sqb = sb("sqb", [128, 8, T], BF16)
    wring = sb("wring", [128, 4, 4096], BF16)
    xa_ext = [sb(f"xa_ext{i}", [128, T + 3]) for i in range(2)]
    hist = sb("hist", [128, 4, 3])
    xc = [sb(f"xc{i}", [128, T]) for i in range(2)]
    xcb = [sb(f"xcb{i}", [128, T], BF16) for i in range(2)]
    L2 = [sb(f"L2_{i}", [128, T]) for i in range(2)]
    L3 = [sb(f"L3_{i}", [128, T]) for i in range(2)]
    L4 = [sb(f"L4_{i}", [128, T]) for i in range(2)]
    L5 = [sb(f"L5_{i}", [128, T]) for i in range(2)]
    gg = [sb(f"gg{i}", [128, T]) for i in range(2)]
    hstate = sb("hstate", [128, 4])
    yaT = sb("yaT", [128, 4, T], BF16)
    ybT = sb("ybT", [128, 4, T], BF16)
    hidT = sb("hidT", [128, 32, T], BF16)
    hflat = hidT.rearrange("p f t -> p (f t)")

    def ov(f0, nf, dt, key):
        v = hflat[:, f0 * T:(f0 + nf) * T]
        if dt == F32:
            v = v.bitcast(F32)
        for f in range(f0, f0 + nf):
            S.add_alias(key, ("hid", f))
        return v

    q_r = [ov(0 + t, 1, BF16, ("q_r", t)) for t in range(4)]
    k_r = [ov(4 + t, 1, BF16, ("k_r", t)) for t in range(4)]
    kdc = [ov(8 + t, 1, BF16, ("kdc", t)) for t in range(4)]
    v_t = [ov(12 + t, 1, BF16, ("v_t", t)) for t in range(4)]
    sgr = [ov(16 + 2 * t, 2, F32, ("sgr", t)) for t in range(4)]
    rt1 = ov(24, 2, F32, "rt1")
    rt2 = ov(26, 2, F32, "rt2")
    qTs = sb("qTs", [128, 4, 128], BF16)
    qTd = [sb(f"qTd{i}", [128, 4, 128], BF16) for i in range(2)]
    kTs = sb("kTs", [128, 4, 128], BF16)
    scT = [sb(f"scT{i}", [128, 2, 4, 128], BF16) for i in range(2)]
    sqr = ov(28, 2, F32, "sqr")
    tmp2 = ov(30, 2, F32, "tmp2")
    ybt = sb("ybt", [128, 512], BF16)
    ssq = sb("ssq", [128, 8])
    st32 = sb("st32", [128, 4, 128])
    stbf = [sb(f"stbf{i}", [128, 4, 128], BF16) for i in range(2)]
    kvt = sb("kvt", [128, 4, 128])
    siga = [sb(f"siga{i}", [128, T]) for i in range(2)]
    sigb = [sb(f"sigb{i}", [128, T]) for i in range(2)]
    mT = sb("mT", [128, 8, T], BF16)
    rl = [sb(f"rl{i}", [128, T]) for i in range(2)]
    cos_s = [sb(f"cos_s{i}", [128, 4, 64]) for i in range(2)]
    sin_s = [sb(f"sin_s{i}", [128, 4, 64]) for i in range(2)]
    pk1 = sb("pk1", [128, PK1])
    pk2 = sb("pk2", [128, PK2])
    g1, g2, gf = pk1[:, 0:8], pk1[:, 8:16], pk1[:, 16:24]
    convw = pk1[:, 24:40].rearrange("p (j k) -> p j k", j=4)
    convb, br, bi, lam = pk1[:, 40:44], pk1[:, 44:48], pk1[:, 48:52], pk1[:, 52:56]
    flag = pk1[:, 56:57]
    kdec = pk1[:, 64:72]
    kdecN = pk1[:, 72:200].rearrange("p (n h) -> p n h", n=16)
    dtab = pk2[:, 0:1024].rearrange("p (a j n) -> p a j n", a=2, j=4)
    qdec = pk2[:, 1024:1536].rearrange("p (j n) -> p j n", j=4)
    cdt = pk2[:, 1536:2048].rearrange("p (j n) -> p j n", j=4)
    maskbd = pk2[:, 2048:2560].rearrange("p (j n) -> p j n", j=4)
    ident = sb("ident", [128, 128], BF16)
    ones = sb("ones", [128, 128], BF16)
    n8sp = sb("n8sp", [128, 4])
    n16sp = sb("n16sp", [128, 4])
    n4sp = sb("n4sp", [128, 4])
    hbr = sb("hbr", [128, 4])
    hbi = sb("hbi", [128, 4])
    qtrc = sb("qtrc", [128, 1])
    epsc = sb("epsc", [128, 1])
    onec = sb("onec", [128, 1])
    wr32 = rt1.rearrange("p (j n) -> p j n", j=4)
    wi32 = rt2.rearrange("p (j n) -> p j n", j=4)
    wrbd = sb("wrbd", [128, 4, 128], BF16)
    wibd = sb("wibd", [128, 4, 128], BF16)

    banks = [nc.alloc_psum_tensor(f"ps{b}", [128, 512], F32).ap() if b != 3 else None for b in range(8)]
    pT_all = nc.alloc_psum_tensor("ps3", [128, 8, 128], BF16).ap()
    rot = [0]
    ROT = [0, 1, 2, 4, 5]

    def nextbank():
        b = ROT[rot[0] % 5]
        rot[0] += 1
        return b

    def bk(b):
        return ("ps", b)

    def dma(eng, out, in_, reads, writes):
        S.add(eng, lambda e, o=out, i=in_: e.dma_start(out=o, in_=i), reads, writes, dma=True)

    def act(out, in_, func, reads, writes, bias=None, scale=None):
        kw = {}
        if bias is not None:
            kw["bias"] = bias
        if scale is not None:
            kw["scale"] = scale
        S.add("act", lambda e, o=out, i=in_, f=func, kw=kw: e.activation(out=o, in_=i, func=f, **kw),
              reads, writes)

    def tt(eng, out, in0, in1, op, reads, writes):
        S.add(eng, lambda e, o=out, a=in0, b=in1, p=op: e.tensor_tensor(out=o, in0=a, in1=b, op=p),
              reads, writes)

    def ts(eng, out, in0, s1, op0, reads, writes, s2=None, op1=None):
        if op1 is None:
            S.add(eng, lambda e, o=out, a=in0, s=s1, p=op0: e.tensor_scalar(out=o, in0=a, scalar1=s, scalar2=None, op0=p),
                  reads, writes)
        else:
            S.add(eng, lambda e, o=out, a=in0, s=s1, p=op0, s_2=s2, p1=op1:
                  e.tensor_scalar(out=o, in0=a, scalar1=s, scalar2=s_2, op0=p, op1=p1), reads, writes)

    def stt(out, in0, scalar, in1, op0, op1, reads, writes):
        S.add("dve", lambda e, o=out, a=in0, s=scalar, b=in1, p0=op0, p1=op1:
              e.scalar_tensor_tensor(out=o, in0=a, scalar=s, in1=b, op0=p0, op1=p1), reads, writes)

    def copy(eng, out, in_, reads, writes):
        if eng == "act":
            S.add("act", lambda e, o=out, i=in_: e.copy(out=o, in_=i), reads, writes)
        else:
            S.add(eng, lambda e, o=out, i=in_: e.tensor_copy(out=o, in_=i), reads, writes)

    def mm_group(out, okey, pairs, reads):
        n = len(pairs)
        for i, pr in enumerate(pairs):
            l, r = pr[0], pr[1]
            rk = list(reads) + (list(pr[2]) if len(pr) > 2 else [])
            S.add("pe", lambda e, o=out, l=l, r=r, st=(i == 0), sp=(i == n - 1):
                  e.matmul(o, lhsT=l, rhs=r, start=st, stop=sp), rk, [okey])

    def mm1(out, okey, l, r, reads, start=True, stop=True):
        S.add("pe", lambda e, o=out, l=l, r=r, st=start, sp=stop: e.matmul(o, lhsT=l, rhs=r, start=st, stop=sp),
              reads, [okey])

    def transp(out, okey, in_, reads):
        S.add("pe", lambda e, o=out, i=in_: e.transpose(o, i, ident), list(reads) + ["ident"], [okey])

    def v3(ap, a):
        return ap.rearrange("p (a b) -> p a b", a=a)

    def blk_src(b):
        dst = scr[b]
        if b < 10:
            return [(v3(dst, 8), w_in.rearrange("(c p) n -> p c n", p=128)[:, :, b * 512:(b + 1) * 512])]
        if b < 12:
            k = b - 10
            d4 = dst.rearrange("p (s c n) -> p s c n", s=2, c=4)
            return [(d4[:, 0], w_a.rearrange("(c p) n -> p c n", p=128)[:, :, k * 512:(k + 1) * 512]),
                    (d4[:, 1], w_b.rearrange("(c p) n -> p c n", p=128)[:, :, k * 512:(k + 1) * 512])]
        if b < 14:
            k = b - 12
            return [(v3(dst, 8), w_out.rearrange("(c p) n -> p c n", p=128)[:, :, k * 512:(k + 1) * 512])]
        if b < 22:
            k = b - 14
            return [(v3(dst, 8), w1.rearrange("(c p) n -> p c n", p=128)[:, :, k * 512:(k + 1) * 512])]
        j = b - 22
        d3 = v3(dst, 32)
        s3 = w2.rearrange("(f p) n -> p f n", p=128)[:, :, j * 128:(j + 1) * 128]
        return [(d3[:, q * 8:(q + 1) * 8, :], s3[:, q * 8:(q + 1) * 8, :]) for q in range(4)]

    dbg = dbg or set()
    cast_rest = [1, 2, 5, 10, 6, 8, 11, 7, 9] + list(range(12, 30))

    def cast_block(b):
        if b >= 22:
            q = b - 22
            dma("pool", scrw2[:, q * 4:(q + 1) * 4, :],
                w2.rearrange("(f p) n -> p f n", p=128)[:, q * 4:(q + 1) * 4, :], [], [("scrw2", q)])
            return
        for (dv, sv) in blk_src(b):
            dma("pool", dv, sv, [], [("scr", b)])

    def cast_quantum():
        if cast_rest:
            cast_block(cast_rest.pop(0))

    for b in (0, 3, 4):
        cast_block(b)

    ring_free = [0, 1, 2, 3]

    def wload(b):
        s = ring_free.pop(0)
        if b >= 22:
            j = b - 22
            dma("sp", wring[:, s, :].rearrange("p (f n) -> p f n", f=32), scrw2[:, :, j * 128:(j + 1) * 128],
                [("scrw2", q) for q in range(8)], [("w", s)])
        else:
            dma("sp", wring[:, s, :], scr[b], [("scr", b)], [("w", s)])
        return wring[:, s, :], ("w", s), s

    def wfree(s):
        ring_free.append(s)

    dma("sp", xT[0], xT_d.rearrange("(c p) t -> p c t", p=128)[:, :, 0:T], [], [("xT", 0, c) for c in range(8)])
    dma("sp", cos_s[0], cos_d[:, 0:4, :], [], [("cos", 0)])
    dma("sp", sin_s[0], sin_d[:, 0:4, :], [], [("sin", 0)])
    dma("sp", pk1, pk1_d, [], ["g1", "g2", "gf", "convw", "convb", "br", "bi", "lam", "flag", "kdec", "kdecN"])
    dma("sp", wr32, wr_d, [], ["rt1"])
    dma("sp", wi32, wi_d, [], ["rt2"])
    dma("sp", pk2, pk2_d, [], ["dtab", "qdec", "cdt", "maskbd"])
    dma("pool", ident, ident_d, [], ["ident"])
    S.add("dve", lambda e: e.memset(ones, 1.0 / 1024.0), [], ["ones"])
    S.add("dve", lambda e: e.memset(epsc, EPS), [], ["epsc"])
    S.add("dve", lambda e: e.memset(onec, 1.0), [], ["onec"])
    S.add("dve", lambda e: e.memset(hist, 0.0), [], ["hist"])
    S.add("dve", lambda e: e.memset(hstate, 0.0), [], ["hstate"])
    S.add("dve", lambda e: e.memset(st32, 0.0), [], ["st32"])
    S.add("dve", lambda e: e.memset(stbf[0], 0.0), [], [("stbf", 0)])
    copy("dve", wrbd, wr32, ["rt1"], ["wrbd"])
    copy("dve", wibd, wi32, ["rt2"], ["wibd"])
    act(n8sp, lam, AF.Exp, ["lam"], ["n8sp"], scale=-1.0)
    act(n8sp, n8sp, AF.Ln, ["n8sp", "onec"], ["n8sp"], bias=onec[:, 0:1], scale=1.0)
    ts("dve", n16sp, n8sp, -16.0, ALU.mult, ["n8sp"], ["n16sp"])
    ts("dve", n4sp, n8sp, -4.0, ALU.mult, ["n8sp"], ["n4sp"])
    ts("dve", n8sp, n8sp, -8.0, ALU.mult, ["n8sp", "n16sp", "n4sp"], ["n8sp"])
    ts("dve", hbr, br, 0.5, ALU.mult, ["br"], ["hbr"])
    ts("dve", hbi, bi, 0.5, ALU.mult, ["bi"], ["hbi"])
    S.add("dve", lambda e: e.memset(qtrc, 0.25), [], ["qtrc"])

    def xkeys(xb):
        return [("xT", xb, c) for c in range(8)]

    def hkeys():
        return [("hT", c) for c in range(8)]

    def load_x(st_idx):
        xb = st_idx % 2
        src = xT_d.rearrange("(c p) t -> p c t", p=128)[:, :, st_idx * T:(st_idx + 1) * T]
        dma("sp", xT[xb], src, [], xkeys(xb))
        cb = st_idx % 2
        dma("sp", cos_s[cb], cos_d[:, st_idx * 4:(st_idx + 1) * 4, :], [], [("cos", cb)])
        dma("sp", sin_s[cb], sin_d[:, st_idx * 4:(st_idx + 1) * 4, :], [], [("sin", cb)])

    def norm_stats_chunk(xb, c, sbank):
        act(sqb[:, c, :], xT[xb][:, c, :], AF.Square, [("xT", xb, c)], [("sq", c)])
        S.add("pe", lambda e, c=c, sbank=sbank: e.matmul(banks[sbank], lhsT=ones, rhs=sqb[:, c, :],
                                                         start=(c == 0), stop=(c == 7)),
              ["ones", ("sq", c)], [bk(sbank)])

    def norm_finish(sbank, w):
        act(rstd2[w], banks[sbank], AF.Ln, [bk(sbank), "epsc"], [("rstd", w)], bias=epsc[:, 0:1], scale=1.0)
        act(rstd2[w], rstd2[w], AF.Exp, [("rstd", w)], [("rstd", w)], scale=-0.5)

    def norm_apply(xb, gcol, gkey, w, to_x, after=None):
        for c in range(8):
            xk = ("xT", xb, c)
            if to_x:
                stt(xT[xb][:, c, :], xT[xb][:, c, :], gcol[:, c:c + 1], rstd2[w], ALU.mult, ALU.mult,
                    [xk, gkey, ("rstd", w)], [xk])
                if after is not None:
                    after(c)
            else:
                stt(hT[:, c, :], xT[xb][:, c, :], gcol[:, c:c + 1], rstd2[w], ALU.mult, ALU.mult,
                    [xk, gkey, ("rstd", w)], [("hT", c)])

    def norm1_stats(st_idx):
        xb = st_idx % 2
        for c in range(8):
            norm_stats_chunk(xb, c, 6)
        norm_finish(6, 1)

    tokbank = [0]

    def tok_unit(nm, t, wb_, kw, main, cb, st_idx, bpair=(5, 7)):
        pb = bpair[tokbank[0] % len(bpair)]
        tokbank[0] += 1
        mm_group(banks[pb], bk(pb), [(hT[:, c, t * 128:(t + 1) * 128], wb_[:, c, :], [("hT", c)]) for c in range(8)],
                 [kw])
        P3 = banks[pb].rearrange("p (h d) -> p h d", h=8)
        if nm in ("q", "k"):
            dst = q_r[t] if nm == "q" else k_r[t]
            dkey = ("q_r", t) if nm == "q" else ("k_r", t)
            cs = cos_s[cb][:, t, :].unsqueeze(1).broadcast_to([128, 8, 64])
            s_lo = sin_s[cb][:, t, 0:32].unsqueeze(1).broadcast_to([128, 8, 32])
            s_hi = sin_s[cb][:, t, 32:64].unsqueeze(1).broadcast_to([128, 8, 32])
            r1 = rt1.rearrange("p (h d) -> p h d", h=8)
            r2 = rt2.rearrange("p (h d) -> p h d", h=8)
            tt("dve", r1, P3, cs, ALU.mult, [bk(pb), ("cos", cb)], ["rt1"])
            tt("dve", r2[:, :, 0:32], P3[:, :, 32:64], s_lo, ALU.mult, [bk(pb), ("sin", cb)], ["rt2"])
            tt("dve", r2[:, :, 32:64], P3[:, :, 0:32], s_hi, ALU.mult, [bk(pb), ("sin", cb)], ["rt2"])
            tt("pool", dst, rt1, rt2, ALU.add, ["rt1", "rt2"], [dkey])
            if nm == "k":
                if main:
                    kd3 = kdec.unsqueeze(2).broadcast_to([128, 8, 64])
                else:
                    kd3 = kdecN[:, st_idx * 4 + t, :].unsqueeze(2).broadcast_to([128, 8, 64])
                tt("pool", kdc[t].rearrange("p (h d) -> p h d", h=8),
                   k_r[t].rearrange("p (h d) -> p h d", h=8), kd3, ALU.mult,
                   [("k_r", t), "kdec", "kdecN"], [("kdc", t)])
        elif nm == "v":
            copy("act", v_t[t], banks[pb], [bk(pb)], [("v_t", t)])
        else:
            act(sgr[t], banks[pb], AF.Silu, [bk(pb)], [("sgr", t)])

    def lru_pieces(main, st_idx):
        W = {}

        def load():
            w_, k_, sl = wload(0)
            W["xa"] = (v3(w_, 8), k_, sl)
            if main:
                w_, k_, sl = wload(1)
                W["ga"] = (v3(w_, 8), k_, sl)

        def A(j):
            jb = j % 2
            wxa, kxa, _ = W["xa"]
            mm_group(banks[0], bk(0), [(wxa[:, c, j * 128:(j + 1) * 128], hT[:, c, :], [("hT", c)]) for c in range(8)],
                     [kxa])
            copy("act", xa_ext[jb][:, 3:T + 3], banks[0], [bk(0)], [("xa_ext", jb)])
            copy("pool", xa_ext[jb][:, 0:3], hist[:, j, :], ["hist"], [("xa_ext", jb)])
            ts("dve", xc[jb], xa_ext[jb][:, 0:T], convw[:, j, 0:1], ALU.mult,
               [("xa_ext", jb), "convw", "convb"], [("xc", jb)], s2=convb[:, j:j + 1], op1=ALU.add)
            for k in range(1, 4):
                stt(xc[jb], xa_ext[jb][:, k:k + T], convw[:, j, k:k + 1], xc[jb], ALU.mult, ALU.add,
                    [("xa_ext", jb), "convw", ("xc", jb)], [("xc", jb)])
            copy("pool", hist[:, j, :], xa_ext[jb][:, T:T + 3], [("xa_ext", jb)], ["hist"])
            copy("dve", xcb[jb], xc[jb], [("xc", jb)], [("xcb", jb)])
            if main:
                wga, kga, _ = W["ga"]
                mm_group(banks[4], bk(4), [(wga[:, c, j * 128:(j + 1) * 128], hT[:, c, :], [("hT", c)]) for c in range(8)],
                         [kga])
                act(gg[jb], banks[4], AF.Gelu_apprx_tanh, [bk(4)], [("gg", jb)])
            if j == 3:
                wfree(W["xa"][2])
                if main:
                    wfree(W["ga"][2])

        def B(j):
            jb = j % 2
            mm1(banks[2], bk(2), wrbd[:, j, :], xcb[jb], ["wrbd", ("xcb", jb)])
            mm1(banks[6], bk(6), wibd[:, j, :], xcb[jb], ["wibd", ("xcb", jb)])
            act(L2[jb], banks[2], AF.Tanh, [bk(2), "hbr"], [("L2", jb)], bias=hbr[:, j:j + 1], scale=0.5)
            act(L3[jb], banks[6], AF.Tanh, [bk(6), "hbi"], [("L3", jb)], bias=hbi[:, j:j + 1], scale=0.5)
            act(L4[jb], L2[jb], AF.Exp, [("L2", jb), "n4sp"], [("L4", jb)], bias=n4sp[:, j:j + 1],
                scale=n4sp[:, j:j + 1])
            act(L2[jb], L2[jb], AF.Exp, [("L2", jb), "n8sp"], [("L2", jb)], bias=n8sp[:, j:j + 1],
                scale=n8sp[:, j:j + 1])
            act(L2[jb], L2[jb], AF.Sqrt, [("L2", jb), "qtrc"], [("L2", jb)], bias=qtrc[:, 0:1], scale=-0.25)
            stt(L3[jb], L3[jb], 1.0, xc[jb], ALU.add, ALU.mult, [("L3", jb), ("xc", jb)], [("L3", jb)])
            tt("pool", L3[jb], L3[jb], L2[jb], ALU.mult, [("L3", jb), ("L2", jb)], [("L3", jb)])
            S.add("dve", lambda e, jb=jb, j=j: e.tensor_tensor_scan(
                out=L5[jb], data0=L4[jb], data1=L3[jb], initial=hstate[:, j:j + 1], op0=ALU.mult, op1=ALU.add),
                [("L4", jb), ("L3", jb), "hstate"], [("L5", jb)])
            copy("pool", hstate[:, j:j + 1], L5[jb][:, T - 1:T], [("L5", jb)], ["hstate"])
            if main:
                tt("pool", yaT[:, j, :], gg[jb], L5[jb], ALU.mult, [("gg", jb), ("L5", jb)], ["yaT"])
                if st_idx == NST:
                    cast_quantum()

        return load, A, B

    def tok_blocks(names):
        bid = {"q": 2, "k": 3, "v": 4, "gr": 5}
        out = []
        for nm in names:
            w_, k_, sl = wload(bid[nm])
            out.append((nm, v3(w_, 8), k_, sl))
        return out

    def lru_standalone(main, st_idx, cb):
        load, A, B = lru_pieces(main, st_idx)
        load()
        fw = tok_blocks(("q", "v") if main else ("k", "v"))
        for j in range(5):
            if j < 4:
                A(j)
                for (nm, w_, k_, sl) in fw:
                    tok_unit(nm, j, w_, k_, main, cb, st_idx, bpair=(5, 7, 1) if main else (5, 7, 4))
            if j >= 1:
                B(j - 1)
        for (nm, w_, k_, sl) in fw:
            wfree(sl)
        if main:
            rest = tok_blocks(("k", "gr"))
            for t in range(4):
                for (nm, w_, k_, sl) in rest:
                    tok_unit(nm, t, w_, k_, main, cb, st_idx, bpair=(5, 7, 1, 0))
            for (nm, w_, k_, sl) in rest:
                wfree(sl)
        else:
            cast_quantum()
            cast_quantum()
            cast_quantum()

    PEND = []

    def tok_phase(st_idx, cb):
        blks = tok_blocks(("q", "v", "k", "gr"))
        for t in range(4):
            for (nm, w_, k_, sl) in blks:
                tok_unit(nm, t, w_, k_, True, cb, st_idx, bpair=(0, 1, 2))
            if t == 0 and PEND:
                PEND.pop(0)()
            if t >= 1:
                ret_A(t - 1, (t - 1) % 2)
            if t >= 2:
                ret_B(t - 2, (t - 2) % 2)
        for (nm, w_, k_, sl) in blks:
            wfree(sl)
        ret_A(3, 1)
        ret_B(2, 0)
        ret_B(3, 1)

    stv = [0]

    def kv_update(t):
        cur = stv[0] % 2
        nxt = 1 - cur
        kvp = banks[7].rearrange("p (j n) -> p j n", j=4)
        tt("pool", st32, st32, cdt, ALU.mult, ["st32", "cdt"], ["st32"])
        for j in range(4):
            mm1(kvp[:, j, :], bk(7), kdc[t][:, j * 128:(j + 1) * 128], v_t[t][:, j * 128:(j + 1) * 128],
                [("kdc", t), ("v_t", t)])
        tt("dve", kvt, kvp, maskbd, ALU.mult, [bk(7), "maskbd"], ["kvt"])
        tt("dve", st32, st32, kvt, ALU.add, ["st32", "kvt"], ["st32"])
        copy("act", stbf[nxt], st32, ["st32"], [("stbf", nxt)])
        stv[0] += 1

    def kv_prepass():
        kvp = banks[7].rearrange("p (j n) -> p j n", j=4)
        for j in range(4):
            for t in range(4):
                mm1(kvp[:, j, :], bk(7), kdc[t][:, j * 128:(j + 1) * 128], v_t[t][:, j * 128:(j + 1) * 128],
                    [("kdc", t), ("v_t", t)], start=(t == 0), stop=(t == 3))
        tt("dve", kvt, kvp, maskbd, ALU.mult, [bk(7), "maskbd"], ["kvt"])
        tt("dve", st32, st32, kvt, ALU.add, ["st32", "kvt"], ["st32"])

    def ret_A(t, sci):
        pT = pT_all
        for j in range(4):
            transp(pT[:, j, :], bk(3), q_r[t][:, j * 128:(j + 1) * 128], [("q_r", t)])
        for j in range(4):
            transp(pT[:, 4 + j, :], bk(3), k_r[t][:, j * 128:(j + 1) * 128], [("k_r", t)])
        copy("act", qTs, pT[:, 0:4, :], [bk(3)], ["qTs"])
        copy("act", kTs, pT[:, 4:8, :], [bk(3)], ["kTs"])
        tt("dve", qTd[sci], pT[:, 0:4, :], qdec, ALU.mult, [bk(3), "qdec"], [("qTd", sci)])
        for h in range(8):
            par, j = h % 2, h // 2
            o = par * 64
            mm1(banks[4 + par][:, j * 128:(j + 1) * 128], bk(4 + par), kTs[o:o + 64, j, :], qTs[o:o + 64, j, :],
                ["kTs", "qTs"])
        sc = scT[sci]
        for par in range(2):
            tt("dve", sc[:, par].rearrange("p j n -> p (j n)"), banks[4 + par],
               dtab[:, par].rearrange("p j n -> p (j n)"), ALU.mult, [bk(4 + par), "dtab"], [("scT", sci)])

    def ret_B(t, sci):
        cur = stv[0] % 2
        pT = pT_all
        sc = scT[sci]
        for j in range(4):
            mm1(banks[6][:, j * 128:(j + 1) * 128], bk(6), qTd[sci][:, j, :], stbf[cur][:, j, :],
                [("qTd", sci), ("stbf", cur)], start=True, stop=False)
            for par in range(2):
                h = 2 * j + par
                mm1(banks[6][:, h * 64:(h + 1) * 64], bk(6), sc[:, par, j, :], v_t[t][:, h * 64:(h + 1) * 64],
                    [("scT", sci), ("v_t", t)], start=False, stop=(par == 1))
        kv_update(t)
        act(sqr, banks[6], AF.Square, [bk(6)], ["sqr"])
        S.add("dve", lambda e: e.tensor_reduce(out=ssq, in_=sqr.rearrange("p (h d) -> p h d", h=8),
                                               op=ALU.add, axis=AX.X), ["sqr"], ["ssq"])
        act(ssq, ssq, AF.Ln, ["ssq", "epsc"], ["ssq"], bias=epsc[:, 0:1], scale=1.0 / 64.0)
        act(ssq, ssq, AF.Exp, ["ssq"], ["ssq"], scale=-0.5)
        tt("dve", tmp2.rearrange("p (h d) -> p h d", h=8), banks[6].rearrange("p (h d) -> p h d", h=8),
           ssq.unsqueeze(2).broadcast_to([128, 8, 64]), ALU.mult, [bk(6), "ssq"], ["tmp2"])
        tt("pool", ybt, tmp2, sgr[t], ALU.mult, ["tmp2", ("sgr", t)], ["ybt"])
        for j in range(4):
            transp(pT[:, j, :], bk(3), ybt[:, j * 128:(j + 1) * 128], ["ybt"])
        copy("act", ybT[:, :, t * 128:(t + 1) * 128], pT[:, 0:4, :], [bk(3)], ["ybT"])

    def retention():
        for f, t, b in ((ret_A, 0, 0), (ret_A, 1, 1), (ret_B, 0, 0), (ret_A, 2, 0), (ret_B, 1, 1), (ret_A, 3, 1),
                        (ret_B, 2, 0), (ret_B, 3, 1)):
            f(t, b)
            cast_quantum()

    def merge(xb):
        for half in range(2):
            wab, kab, sl_ab = wload(10 + half)
            wab = wab.rearrange("p (s c n) -> p s c n", s=2, c=4)
            wsa, ksa, sl_sa = wload(6 + half)
            wsa = v3(wsa, 8)
            wsb, ksb, sl_sb = wload(8 + half)
            wsb = v3(wsb, 8)
            for jj in range(4):
                j = half * 4 + jj
                jb = j % 2
                cs = slice(jj * 128, (jj + 1) * 128)
                bsa, bsb, bpa, bpb = nextbank(), nextbank(), nextbank(), nextbank()
                mm_group(banks[bsa], bk(bsa), [(wsa[:, c, cs], hT[:, c, :], [("hT", c)]) for c in range(8)], [ksa])
                mm_group(banks[bsb], bk(bsb), [(wsb[:, c, cs], hT[:, c, :], [("hT", c)]) for c in range(8)], [ksb])
                mm_group(banks[bpa], bk(bpa), [(wab[:, 0, c, cs], yaT[:, c, :]) for c in range(4)], [kab, "yaT"])
                mm_group(banks[bpb], bk(bpb), [(wab[:, 1, c, cs], ybT[:, c, :]) for c in range(4)], [kab, "ybT"])
                act(siga[jb], banks[bsa], AF.Sigmoid, [bk(bsa)], [("siga", jb)])
                act(sigb[jb], banks[bsb], AF.Sigmoid, [bk(bsb)], [("sigb", jb)])
                tt("dve", siga[jb], banks[bpa], siga[jb], ALU.mult, [bk(bpa), ("siga", jb)], [("siga", jb)])
                tt("dve", sigb[jb], banks[bpb], sigb[jb], ALU.mult, [bk(bpb), ("sigb", jb)], [("sigb", jb)])
                tt("pool", mT[:, j, :], siga[jb], sigb[jb], ALU.add, [("siga", jb), ("sigb", jb)], [("mT", j)])
            wfree(sl_ab)
            wfree(sl_sa)
            wfree(sl_sb)
            cast_quantum()
            cast_quantum()
        for half in range(2):
            wo, ko, sl_o = wload(12 + half)
            wo = v3(wo, 8)
            for jj in range(4):
                j = half * 4 + jj
                pb = nextbank()
                mm_group(banks[pb], bk(pb), [(wo[:, c, jj * 128:(jj + 1) * 128], mT[:, c, :], [("mT", c)]) for c in range(8)],
                         [ko])
                tt("dve", xT[xb][:, j, :], banks[pb], xT[xb][:, j, :], ALU.add, [bk(pb), ("xT", xb, j)],
                   [("xT", xb, j)])
                if j >= 1:
                    norm_stats_chunk(xb, j - 1, 7)
            wfree(sl_o)
        norm_stats_chunk(xb, 7, 7)
        norm_finish(7, 0)

    LRU_SCHED = [[("A", 0)], [("A", 1)], [("B", 0)], [("A", 2)], [("B", 1)], [("A", 3)], [("B", 2)], [("B", 3)]]

    def ffn(xb, st_idx):
        n = 0
        for b in range(8):
            wv, kw, sl = wload(14 + b)
            wv = v3(wv, 8)
            for i in range(4):
                pb = nextbank()
                rb = n % 2
                n += 1
                mm_group(banks[pb], bk(pb), [(wv[:, c, i * 128:(i + 1) * 128], hT[:, c, :], [("hT", c)]) for c in range(8)],
                         [kw])
                act(rl[rb], banks[pb], AF.Relu, [bk(pb)], [("rl", rb)])
                tt("pool", hidT[:, b * 4 + i, :], rl[rb], rl[rb], ALU.mult, [("rl", rb)], [("hid", b * 4 + i)])
            wfree(sl)
            if b == 4 and st_idx + 1 < n_st:
                norm1_stats(st_idx + 1)
        overlap = st_idx + 1 < n_st
        if overlap:
            norm_apply((st_idx + 1) % 2, g1, "g1", 1, to_x=False)
            load, A, B = lru_pieces(True, st_idx + 1)
        for j in range(8):
            wv, kw, sl = wload(22 + j)
            wv = v3(wv, 32)
            pb = (1, 5)[j % 2]
            mm_group(banks[pb], bk(pb), [(wv[:, f, :], hidT[:, f, :], [("hid", f)]) for f in range(32)], [kw])
            wfree(sl)
            tt("dve", xT[xb][:, j, :], banks[pb], xT[xb][:, j, :], ALU.add, [bk(pb), ("xT", xb, j)],
               [("xT", xb, j)])
            if j >= 1:
                norm_stats_chunk(xb, j - 1, 7)
            if overlap:
                if j == 0:
                    load()
                for kind, jj in LRU_SCHED[j]:
                    (A if kind == "A" else B)(jj)
        norm_stats_chunk(xb, 7, 7)
        norm_finish(7, 0)

    sci = [0]
    norm1_stats(0)
    for st_idx in range(n_st):
        main = st_idx >= NST
        xb = st_idx % 2
        cb = st_idx % 2
        if st_idx == NST:
            while cast_rest and cast_rest[0] < 14:
                cast_quantum()
            ts("dve", hstate, hstate, flag[:, 0:1], ALU.mult, ["hstate", "flag"], ["hstate"])
        if st_idx == 0:
            norm_apply(xb, g1, "g1", 1, to_x=False)
        if st_idx <= NST:
            lru_standalone(main, st_idx, cb)
        else:
            tok_phase(st_idx, cb)
        if st_idx + 1 < n_st:
            load_x(st_idx + 1)
        if main:
            if st_idx == NST:
                retention()
            merge(xb)
            norm_apply(xb, g2, "g2", 0, to_x=False)
            while cast_rest:
                cast_quantum()
            ffn(xb, st_idx)
            m = st_idx - NST
            dst = outT_d.rearrange("(c p) t -> p c t", p=128)[:, :, m * T:(m + 1) * T]
            if st_idx == n_st - 1:
                norm_apply(xb, gf, "gf", 0, to_x=True,
                           after=lambda c: dma("act", dst[:, c, :], xT[xb][:, c, :], [("xT", xb, c)], [("out", m, c)]))
            else:
                norm_apply(xb, gf, "gf", 0, to_x=True)
                PEND.append(lambda dst=dst, xb=xb, m=m: dma("act", dst, xT[xb], xkeys(xb), [("out", m)]))
        else:
            if st_idx + 1 < n_st:
                norm1_stats(st_idx + 1)
                norm_apply((st_idx + 1) % 2, g1, "g1", 1, to_x=False)
            kv_prepass()
            if st_idx == NST - 1:
                copy("act", stbf[stv[0] % 2], st32, ["st32"], [("stbf", stv[0] % 2)])
        S.new_epoch()
    S.emit()
    return nc


def _const_tables():
    nh, dh, C = 8, 64, 128
    log_g = np.log1p(-np.exp2(-5.0 - np.arange(nh, dtype=np.float64)))
    pos = np.arange(C, dtype=np.float64)
    rel = pos[None, :] - pos[:, None]
    dtab = np.zeros((C, 2, 4, C), np.float64)
    for h in range(nh):
        dtab[:, h % 2, h // 2, :] = np.where(rel >= 0, np.exp(log_g[h] * np.maximum(rel, 0.0)), 0.0) * 0.125
    qdec = np.zeros((128, 4, C), np.float64)
    cdt = np.zeros((128, 4, 128), np.float64)
    mask = np.zeros((128, 4, 128), np.float64)
    for p in range(128):
        for j in range(4):
            h = 2 * j + p // 64
            qdec[p, j, :] = np.exp(log_g[h] * (pos + 1.0))
            cdt[p, j, :] = np.exp(log_g[h] * C)
            mask[p, j, (p // 64) * 64:(p // 64) * 64 + 64] = 1.0
    kdec = np.zeros((C, nh), np.float64)
    for h in range(nh):
        kdec[:, h] = np.exp(log_g[h] * (C - 1.0 - pos)) * 0.125
    kdecN = np.zeros((C, 16, nh), np.float64)
    for n in range(16):
        for h in range(nh):
            kdecN[:, n, h] = np.exp(log_g[h] * (C - 1.0 - pos + C * (15 - n))) * 0.125
    ident = np.eye(128, dtype=np.float32)
    return (dtab.astype(np.float32), qdec.astype(np.float32), kdec.astype(np.float32),
            cdt.astype(np.float32), mask.astype(np.float32), ident, kdecN.astype(np.float32))


def _rope_tables(pos0):
    half = 32
    inv = 10000.0 ** (-np.arange(half, dtype=np.float64) / half)
    p = np.arange(128)[:, None] + np.arange(32)[None, :] * 128 + pos0
    ang = p.astype(np.float64)[:, :, None] * inv[None, None, :]
    c, s = np.cos(ang), np.sin(ang)
    cos2 = np.concatenate([c, c], axis=-1).astype(np.float32)
    sin2 = np.concatenate([-s, s], axis=-1).astype(np.float32)
    return np.ascontiguousarray(cos2), np.ascontiguousarray(sin2)


def kernel(x, norm1_g, w_in, conv_w, conv_b, lru_wr, lru_br, lru_wi, lru_bi, lru_lambda,
           w_branch_a, w_branch_b, w_out, norm2_g, w_ff1, w_ff2, norm_f_g):
    f32 = np.float32
    x = np.asarray(x, f32)

    def pc(v, n):
        return np.ascontiguousarray(np.asarray(v, f32).reshape(n, 128).T)

    dtab, qdec, kdec, cdt, mask, ident, kdecN = _const_tables()
    shared = {
        "w_in": np.ascontiguousarray(np.asarray(w_in, f32)[0]),
        "w_a": np.ascontiguousarray(np.asarray(w_branch_a, f32)[0]),
        "w_b": np.ascontiguousarray(np.asarray(w_branch_b, f32)[0]),
        "w_out": np.ascontiguousarray(np.asarray(w_out, f32)[0]),
        "w1": np.ascontiguousarray(np.asarray(w_ff1, f32)[0]),
        "w2": np.ascontiguousarray(np.asarray(w_ff2, f32)[0]),
    }
    wr_bd = np.zeros((128, 4, 128), f32)
    wi_bd = np.zeros((128, 4, 128), f32)
    wr0, wi0 = np.asarray(lru_wr, f32)[0], np.asarray(lru_wi, f32)[0]
    for blk in range(8):
        j, o = blk // 2, (blk % 2) * 64
        wr_bd[o:o + 64, j, o:o + 64] = wr0[blk]
        wi_bd[o:o + 64, j, o:o + 64] = wi0[blk]
    shared["wr"] = wr_bd
    shared["wi"] = wi_bd
    shared["ident"] = ident
    pk2 = np.concatenate([dtab.reshape(128, -1), qdec.reshape(128, -1), cdt.reshape(128, -1),
                          mask.reshape(128, -1)], axis=1).astype(f32)
    shared["pk2"] = np.ascontiguousarray(pk2)

    def pack1(flagval):
        pk = np.zeros((128, PK1), f32)
        pk[:, 0:8] = pc(np.asarray(norm1_g)[0], 8)
        pk[:, 8:16] = pc(np.asarray(norm2_g)[0], 8)
        pk[:, 16:24] = pc(norm_f_g, 8)
        pk[:, 24:40] = np.asarray(conv_w, f32)[0].reshape(4, 4, 128).transpose(2, 1, 0).reshape(128, 16)
        pk[:, 40:44] = pc(np.asarray(conv_b)[0], 4)
        pk[:, 44:48] = pc(np.asarray(lru_br)[0].reshape(-1), 4)
        pk[:, 48:52] = pc(np.asarray(lru_bi)[0].reshape(-1), 4)
        pk[:, 52:56] = pc(np.asarray(lru_lambda)[0], 4)
        pk[:, 56] = flagval
        pk[:, 64:72] = kdec
        pk[:, 72:200] = kdecN.reshape(128, 128)
        return pk

    in_maps = []
    for c in range(8):
        b, half = c // 2, c % 2
        xt = np.zeros((D, 2 * TOK), f32)
        if half == 1:
            xt[:, :TOK] = x[b, :TOK].T
        xt[:, TOK:] = x[b, half * TOK:(half + 1) * TOK].T
        cos2, sin2 = _rope_tables(0 if half == 1 else -TOK)
        m = dict(shared)
        m["xT"] = xt
        m["pk1"] = pack1(float(half))
        m["cos2"] = cos2
        m["sin2"] = sin2
        in_maps.append(m)
    nc = build_nc()
    res = run_bass_kernel_spmd(nc, in_maps, core_ids=list(range(8)))
    out = np.empty((NB, SEQ, D), f32)
    for c in range(8):
        b, half = c // 2, c % 2
        out[b, half * TOK:(half + 1) * TOK, :] = np.asarray(res.results[c]["outT"], f32).T
    return out
```
